# Optimizing a Trainium2 kernel written in Bass

```python
import jax
import jax.numpy as jnp
from jax import lax
import numpy as np

D_MODEL = 1024
BATCH = 4
SEQ = 8192
DEPTH = 2

GRID_W = 64
CTX_LEN = 256
D_MIX = D_MODEL
HEAD_DIM = 64
ATTN_HEADS = 8
ATTN_KV_HEADS = 2
ATTN_GROUP = ATTN_HEADS // ATTN_KV_HEADS
ATTN_DIM = ATTN_HEADS * HEAD_DIM
KV_DIM = ATTN_KV_HEADS * HEAD_DIM
Q_BLOCK = 128
ROPE_THETA = 10000.0
AXIS_ROT_DIM = HEAD_DIM // 2
AXIS_FREQS = AXIS_ROT_DIM // 2
RWKV_HEADS = 8
RWKV_HEAD_DIM = 64
RWKV_DIM = RWKV_HEADS * RWKV_HEAD_DIM
DECAY_LORA = 64
ICLR_LORA = 64
GATE_LORA = 128
N_DIRS = 2
ATTN_COLS = ATTN_DIM + 2 * KV_DIM
RWKV_COLS = 3 * RWKV_DIM + N_DIRS * (DECAY_LORA + ICLR_LORA) + GATE_LORA
IN_COLS = ATTN_COLS + RWKV_COLS
SHIFT_WIDTH = 3
N_KEYS = 128
N_EXPERTS = N_KEYS * N_KEYS
PEER_HEADS = 8
PEER_KEY_DIM = 256
PEER_HALF = PEER_KEY_DIM // 2
PEER_TOPK = 16
TOKEN_BLOCK = 128
N_MOD = 6
NORM_EPS = 1e-6
GN_EPS = 64e-5
L2_EPS = 1e-12

kernel_name = 'hybrid_gqa_rwkv7_peer_dit'


def rms_norm(x, gain):
    xf = x.astype(jnp.float32)
    y = xf * lax.rsqrt(jnp.mean(xf * xf, axis=-1, keepdims=True) + NORM_EPS)
    return (y * gain.astype(jnp.float32)).astype(x.dtype)


def modulate(h, shift, scale):
    return h * (1 + scale) + shift


def axial_rope_tables(n_tokens):
    rows = n_tokens // GRID_W
    row = jnp.broadcast_to(jnp.arange(rows, dtype=jnp.float32)[:, None], (rows, GRID_W)).reshape(-1)
    col = jnp.broadcast_to(jnp.arange(GRID_W, dtype=jnp.float32)[None, :], (rows, GRID_W)).reshape(-1)
    inv_freq = ROPE_THETA ** (-jnp.arange(AXIS_FREQS, dtype=jnp.float32) * 2.0 / AXIS_ROT_DIM)
    ang = jnp.stack([row[:, None] * inv_freq, col[:, None] * inv_freq], axis=1)
    return jnp.cos(ang), jnp.sin(ang)


def apply_axial_rope(x, cos, sin):
    B, L, H, _ = x.shape
    xr = x.astype(jnp.float32).reshape(B, L, H, 2, 2, AXIS_FREQS)
    x1, x2 = xr[..., 0, :], xr[..., 1, :]
    c, s = cos[None, :, None], sin[None, :, None]
    out = jnp.stack([x1 * c - x2 * s, x2 * c + x1 * s], axis=-2)
    return out.reshape(B, L, H, HEAD_DIM).astype(x.dtype)


def block_attention(q, k, v):
    B, Lq = q.shape[:2]
    n_blk = Lq // Q_BLOCK
    qb = q.reshape(B, n_blk, Q_BLOCK, ATTN_KV_HEADS, ATTN_GROUP, HEAD_DIM).swapaxes(0, 1)
    scale = HEAD_DIM ** -0.5

    def one_block(q_blk):
        s = jnp.einsum('bqkgd,bskd->bkgqs', q_blk, k).astype(jnp.float32) * scale
        p = jax.nn.softmax(s, axis=-1).astype(v.dtype)
        return jnp.einsum('bkgqs,bskd->bqkgd', p, v)

    o = lax.map(one_block, qb)
    return o.swapaxes(0, 1).reshape(B, Lq, ATTN_DIM)


def centred_shift(x, taps):
    xp = jnp.pad(x, ((0, 0), (1, 1), (0, 0)))
    return xp[:, :-2] * taps[0] + xp[:, 1:-1] * taps[1] + xp[:, 2:] * taps[2]


def rwkv_heads(t):
    return t.reshape(t.shape[0], t.shape[1], RWKV_HEADS, RWKV_HEAD_DIM).astype(jnp.float32)


def rwkv_features(cols, shift_taps, decay_base, decay_up, iclr_base, iclr_up, gate_up, k_k, k_a):
    B, L, _ = cols.shape
    cols = centred_shift(cols, shift_taps)
    o3 = 3 * RWKV_DIM
    o4 = o3 + N_DIRS * DECAY_LORA
    o5 = o4 + N_DIRS * ICLR_LORA
    r, k, v, wd, ad, gd = jnp.split(cols, [RWKV_DIM, 2 * RWKV_DIM, o3, o4, o5], axis=-1)
    wd = jnp.tanh(wd).reshape(B, L, N_DIRS, DECAY_LORA)
    ad = ad.reshape(B, L, N_DIRS, ICLR_LORA)
    z = (decay_base + jnp.einsum('bldr,drc->bldc', wd, decay_up)).astype(jnp.float32)
    decay = jnp.exp(-jnp.exp(-jax.nn.softplus(-z) - 0.5))
    iclr = jax.nn.sigmoid((iclr_base + jnp.einsum('bldr,drc->bldc', ad, iclr_up)).astype(jnp.float32))
    gate = jax.nn.sigmoid(gd) @ gate_up
    kk = rwkv_heads(k * k_k)
    kk = kk * lax.rsqrt(jnp.sum(kk * kk, axis=-1, keepdims=True) + L2_EPS)
    k_dir = k[:, :, None, :].astype(jnp.float32) * (1 + (iclr - 1) * k_a.astype(jnp.float32))
    return r, k, v, gate, decay, iclr, kk, k_dir


def wkv_scan(r, w, k, v, kk, b, s0, reverse, emit):
    xs = tuple(jnp.moveaxis(t, 1, 0) for t in (r, w, k, v, kk, b))

    def step(state, inp):
        r_t, w_t, k_t, v_t, kk_t, b_t = inp
        s_kk = jnp.einsum('bhij,bhj->bhi', state, kk_t)
        state = state * w_t[:, :, None, :] - s_kk[..., None] * b_t[:, :, None, :] + v_t[..., None] * k_t[:, :, None, :]
        y = jnp.einsum('bhij,bhj->bhi', state, r_t) if emit else None
        return state, y

    s_final, ys = lax.scan(step, s0, xs, reverse=reverse)
    return s_final, (jnp.moveaxis(ys, 0, 1) if emit else None)


def rwkv_direction(feats, d, s0, reverse, emit):
    r, k, v, gate, decay, iclr, kk, k_dir = feats
    return wkv_scan(rwkv_heads(r), rwkv_heads(decay[:, :, d]), rwkv_heads(k_dir[:, :, d]), rwkv_heads(v),
                    kk, kk * rwkv_heads(iclr[:, :, d]), s0, reverse, emit)


def rwkv_output(y, feats, r_k, ln_w, ln_b):
    r, k, v, gate, decay, iclr, kk, k_dir = feats
    B, L = y.shape[:2]
    mu = jnp.mean(y, axis=-1, keepdims=True)
    var = jnp.mean(jnp.square(y - mu), axis=-1, keepdims=True)
    yn = ((y - mu) * lax.rsqrt(var + GN_EPS)).reshape(B, L, RWKV_DIM)
    yn = yn * ln_w.astype(jnp.float32) + ln_b.astype(jnp.float32)
    bonus = jnp.sum(rwkv_heads(r) * rwkv_heads(k) * r_k.astype(jnp.float32), axis=-1, keepdims=True) * rwkv_heads(v)
    out = (yn + bonus.reshape(B, L, RWKV_DIM)) * gate.astype(jnp.float32)
    return out.astype(r.dtype)


def rwkv_mixer(cols_lat, cols_ctx, shift_taps, decay_base, decay_up, iclr_base, iclr_up, gate_up,
               k_k, k_a, r_k, ln_w, ln_b, need_ctx_out):
    feats_lat = rwkv_features(cols_lat, shift_taps, decay_base, decay_up, iclr_base, iclr_up, gate_up, k_k, k_a)
    feats_ctx = rwkv_features(cols_ctx, shift_taps, decay_base, decay_up, iclr_base, iclr_up, gate_up, k_k, k_a)
    s0 = jnp.zeros((cols_lat.shape[0], RWKV_HEADS, RWKV_HEAD_DIM, RWKV_HEAD_DIM), jnp.float32)
    ys_lat, ys_ctx = [], []
    for d in range(N_DIRS):
        reverse = d == 1
        s_ctx, y_ctx = rwkv_direction(feats_ctx, d, s0, reverse, need_ctx_out)
        _, y_lat = rwkv_direction(feats_lat, d, s_ctx, reverse, True)
        ys_lat.append(y_lat)
        ys_ctx.append(y_ctx)
    out_lat = rwkv_output(ys_lat[0] + ys_lat[1], feats_lat, r_k, ln_w, ln_b)
    if not need_ctx_out:
        return out_lat, None
    out_ctx = rwkv_output(ys_ctx[0] + ys_ctx[1], feats_ctx, r_k, ln_w, ln_b)
    return out_lat, out_ctx


def token_mixer(h_lat, h_ctx, rope_cos, rope_sin, w_in, q_gain, k_gain, shift_taps, decay_base, decay_up,
                iclr_base, iclr_up, gate_up, k_k, k_a, r_k, ln_w, ln_b, w_out, need_ctx_out):
    p_lat = h_lat @ w_in
    p_ctx = h_ctx @ w_in

    def split_attn(p):
        B, L, _ = p.shape
        q = p[..., :ATTN_DIM].reshape(B, L, ATTN_HEADS, HEAD_DIM)
        k = p[..., ATTN_DIM:ATTN_DIM + KV_DIM].reshape(B, L, ATTN_KV_HEADS, HEAD_DIM)
        v = p[..., ATTN_DIM + KV_DIM:ATTN_COLS].reshape(B, L, ATTN_KV_HEADS, HEAD_DIM)
        return q, rms_norm(k, k_gain), v

    q_l, k_l, v_l = split_attn(p_lat)
    q_c, k_c, v_c = split_attn(p_ctx)
    q_l = apply_axial_rope(rms_norm(q_l, q_gain), rope_cos, rope_sin)
    k_l = apply_axial_rope(k_l, rope_cos, rope_sin)
    attn_lat = block_attention(q_l, jnp.concatenate([k_l, k_c], axis=1), jnp.concatenate([v_l, v_c], axis=1))
    rwkv_lat, rwkv_ctx = rwkv_mixer(p_lat[..., ATTN_COLS:], p_ctx[..., ATTN_COLS:], shift_taps, decay_base,
                                    decay_up, iclr_base, iclr_up, gate_up, k_k, k_a, r_k, ln_w, ln_b,
                                    need_ctx_out)
    out_lat = jnp.concatenate([attn_lat, rwkv_lat], axis=-1) @ w_out
    if not need_ctx_out:
        return out_lat, None
    attn_ctx = block_attention(rms_norm(q_c, q_gain), k_c, v_c)
    out_ctx = jnp.concatenate([attn_ctx, rwkv_ctx], axis=-1) @ w_out
    return out_lat, out_ctx


def peer_ffn(h, w_query, subkeys1, subkeys2, expert_u, expert_v):
    B, L, D = h.shape
    q = (h @ w_query).reshape(B, L, PEER_HEADS, 2, PEER_HALF)
    s1 = jnp.einsum('blhd,kd->blhk', q[..., 0, :], subkeys1).astype(jnp.float32)
    s2 = jnp.einsum('blhd,kd->blhk', q[..., 1, :], subkeys2).astype(jnp.float32)
    v1, i1 = lax.top_k(s1, PEER_TOPK)
    v2, i2 = lax.top_k(s2, PEER_TOPK)
    n_cand = PEER_TOPK * PEER_TOPK
    cand_score = (v1[..., :, None] + v2[..., None, :]).reshape(B, L, PEER_HEADS, n_cand)
    cand_index = (i1[..., :, None] * N_KEYS + i2[..., None, :]).reshape(B, L, PEER_HEADS, n_cand)
    top_score, top_pos = lax.top_k(cand_score, PEER_TOPK)
    expert_idx = jnp.take_along_axis(cand_index, top_pos, axis=-1)
    gates = jax.nn.softmax(top_score, axis=-1)
    n_blk = (B * L) // TOKEN_BLOCK
    hb = h.reshape(n_blk, TOKEN_BLOCK, D)
    ib = expert_idx.reshape(n_blk, TOKEN_BLOCK, PEER_HEADS * PEER_TOPK)
    gb = gates.reshape(n_blk, TOKEN_BLOCK, PEER_HEADS * PEER_TOPK)

    def one_block(args):
        h_blk, i_blk, g_blk = args
        u = jnp.take(expert_u, i_blk, axis=0)
        act = jax.nn.gelu(jnp.einsum('td,ted->te', h_blk, u).astype(jnp.float32), approximate=False)
        v = jnp.take(expert_v, i_blk, axis=0)
        return jnp.einsum('te,ted->td', (g_blk * act).astype(v.dtype), v)

    out = lax.map(one_block, (hb, ib, gb))
    return out.reshape(B, L, D)


def setup_inputs(seed: int = 0) -> dict:
    key = jax.random.key(seed)
    ks = jax.random.split(key, 28)

    def nrm(k, shape, s):
        return jax.random.normal(k, shape, jnp.float32) * s

    return {
        'x': nrm(ks[0], (BATCH, SEQ, D_MODEL), 1.0),
        'c': nrm(ks[1], (BATCH, D_MODEL), 1.0),
        'ctx': nrm(ks[2], (BATCH, CTX_LEN, D_MODEL), 1.0),
        'c_ctx': nrm(ks[3], (D_MODEL,), 1.0),
        'mod_w': nrm(ks[4], (DEPTH, D_MODEL, N_MOD * D_MODEL), 0.5 * D_MODEL ** -0.5),
        'mod_b': nrm(ks[5], (DEPTH, N_MOD * D_MODEL), 0.01),
        'norm_mix': 1.0 + nrm(ks[6], (DEPTH, D_MODEL), 0.02),
        'norm_ffn': 1.0 + nrm(ks[7], (DEPTH, D_MODEL), 0.02),
        'w_in': nrm(ks[8], (DEPTH, D_MODEL, IN_COLS), D_MODEL ** -0.5),
        'q_gain': 1.0 + nrm(ks[9], (DEPTH, HEAD_DIM), 0.02),
        'k_gain': 1.0 + nrm(ks[10], (DEPTH, HEAD_DIM), 0.02),
        'shift_taps': jnp.array([0.25, 1.0, 0.25], jnp.float32)[None, :, None]
                      + nrm(ks[11], (DEPTH, SHIFT_WIDTH, RWKV_COLS), 0.05),
        'decay_base': jax.random.uniform(ks[12], (DEPTH, N_DIRS, RWKV_DIM), jnp.float32, -6.0, 2.0),
        'decay_up': nrm(ks[13], (DEPTH, N_DIRS, DECAY_LORA, RWKV_DIM), 0.1),
        'iclr_base': nrm(ks[14], (DEPTH, N_DIRS, RWKV_DIM), 0.5),
        'iclr_up': nrm(ks[15], (DEPTH, N_DIRS, ICLR_LORA, RWKV_DIM), 0.1),
        'gate_up': nrm(ks[16], (DEPTH, GATE_LORA, RWKV_DIM), GATE_LORA ** -0.5),
        'k_k': 0.85 + nrm(ks[17], (DEPTH, RWKV_DIM), 0.05),
        'k_a': 1.0 + nrm(ks[18], (DEPTH, RWKV_DIM), 0.05),
        'r_k': nrm(ks[19], (DEPTH, RWKV_HEADS, RWKV_HEAD_DIM), 0.1),
        'ln_x_w': 1.0 + nrm(ks[20], (DEPTH, RWKV_DIM), 0.02),
        'ln_x_b': nrm(ks[21], (DEPTH, RWKV_DIM), 0.02),
        'w_out': nrm(ks[22], (DEPTH, D_MIX, D_MODEL), D_MIX ** -0.5),
        'peer_query': nrm(ks[23], (DEPTH, D_MODEL, PEER_HEADS * PEER_KEY_DIM), D_MODEL ** -0.5),
        'peer_subkeys1': nrm(ks[24], (DEPTH, N_KEYS, PEER_HALF), PEER_HALF ** -0.5),
        'peer_subkeys2': nrm(ks[25], (DEPTH, N_KEYS, PEER_HALF), PEER_HALF ** -0.5),
        'expert_u': nrm(ks[26], (DEPTH, N_EXPERTS, D_MODEL), D_MODEL ** -0.5),
        'expert_v': nrm(ks[27], (DEPTH, N_EXPERTS, D_MODEL), 0.3),
    }


def reference(x, c, ctx, c_ctx, mod_w, mod_b, norm_mix, norm_ffn, w_in, q_gain, k_gain, shift_taps,
              decay_base, decay_up, iclr_base, iclr_up, gate_up, k_k, k_a, r_k, ln_x_w, ln_x_b, w_out,
              peer_query, peer_subkeys1, peer_subkeys2, expert_u, expert_v):
    rope_cos, rope_sin = axial_rope_tables(x.shape[1])
    for l in range(DEPTH):
        need_ctx = l < DEPTH - 1
        mod_lat = jax.nn.silu(c) @ mod_w[l] + mod_b[l]
        mod_ctx = jax.nn.silu(c_ctx) @ mod_w[l] + mod_b[l]
        sh_m, sc_m, g_m, sh_f, sc_f, g_f = jnp.split(mod_lat[:, None, :], N_MOD, axis=-1)
        csh_m, csc_m, cg_m, csh_f, csc_f, cg_f = jnp.split(mod_ctx, N_MOD, axis=-1)
        h_lat = modulate(rms_norm(x, norm_mix[l]), sh_m, sc_m)
        h_ctx = modulate(rms_norm(ctx, norm_mix[l]), csh_m, csc_m)
        mix_lat, mix_ctx = token_mixer(h_lat, h_ctx, rope_cos, rope_sin, w_in[l], q_gain[l], k_gain[l],
                                       shift_taps[l], decay_base[l], decay_up[l], iclr_base[l], iclr_up[l],
                                       gate_up[l], k_k[l], k_a[l], r_k[l], ln_x_w[l], ln_x_b[l], w_out[l],
                                       need_ctx)
        x = x + g_m * mix_lat
        x = x + g_f * peer_ffn(modulate(rms_norm(x, norm_ffn[l]), sh_f, sc_f), peer_query[l],
                               peer_subkeys1[l], peer_subkeys2[l], expert_u[l], expert_v[l])
        if need_ctx:
            ctx = ctx + cg_m * mix_ctx
            ctx = ctx + cg_f * peer_ffn(modulate(rms_norm(ctx, norm_ffn[l]), csh_f, csc_f), peer_query[l],
                                        peer_subkeys1[l], peer_subkeys2[l], expert_u[l], expert_v[l])
    return x
```

```python
import numpy as np
import concourse.bass as bass
import concourse.mybir as mybir
from concourse.bass_utils import run_bass_kernel_spmd
from contextlib import ExitStack

F32 = mybir.dt.float32
BF16 = mybir.dt.bfloat16
I32 = mybir.dt.int32
AF = mybir.ActivationFunctionType
ALU = mybir.AluOpType
AX = mybir.AxisListType


class Tok:
    __slots__ = ("w", "r", "name")

    def __init__(self, name=""):
        self.w = None
        self.r = {}
        self.name = name


class SyncObj:
    def __init__(self, sem, step, name):
        self.sem = sem
        self.step = step
        self.count = 0
        self.name = name


class Eng:
    def __init__(self, k, name, eng, sem):
        self.k = k
        self.name = name
        self.eng = eng
        self.so = SyncObj(sem, 1, name)
        self.seen = {}
        self.n_dma = 0


class K:
    NDMASEM = 6

    def __init__(self, nc, es):
        self.nc = nc
        self.es = es
        self.es0 = es
        self.engs = {}
        for name, eng in (("pe", nc.tensor), ("act", nc.scalar), ("dve", nc.vector),
                          ("pool", nc.gpsimd), ("sp", nc.sync)):
            sem = es.enter_context(nc.semaphore("s_" + name))
            self.engs[name] = Eng(self, name, eng, sem)
        self.dmasems = {}
        for q in ("sp", "pool", "act"):
            lst = []
            for i in range(self.NDMASEM):
                sem = es.enter_context(nc.semaphore("d_%s%d" % (q, i)))
                lst.append(SyncObj(sem, 16, "d_%s%d" % (q, i)))
            self.dmasems[q] = lst
        self.ninst = 0

    def sb(self, name, shape, dt=F32):
        self.nname = getattr(self, "nname", 0) + 1
        name = "%s_%d" % (name, self.nname)
        t = self.es.enter_context(self.nc.sbuf_tensor(name, list(shape), dt))
        return t

    def ps(self, name, shape, dt=F32):
        t = self.es.enter_context(self.nc.psum_tensor(name, list(shape), dt))
        return t

    def _need(self, reads, writes):
        need = {}

        def add(so, v):
            if need.get(so, 0) < v:
                need[so] = v
        for t in reads:
            if t.w is not None:
                add(*t.w)
        for t in writes:
            if t.w is not None:
                add(*t.w)
            for so, v in t.r.items():
                add(so, v)
        return need

    def _waits(self, e, need):
        for so, v in need.items():
            if e.seen.get(so, 0) < v:
                e.eng.wait_ge(so.sem, v)
                e.seen[so] = v
                self.ninst += 1

    def _mark(self, so, val, reads, writes):
        for t in reads:
            if t.r.get(so, 0) < val:
                t.r[so] = val
        for t in writes:
            t.w = (so, val)
            t.r = {}

    def _emit(self, e, need, make):
        pend = [(so, v) for so, v in need.items() if e.seen.get(so, 0) < v]
        for so, v in pend[:-1]:
            e.eng.wait_ge(so.sem, v)
            e.seen[so] = v
            self.ninst += 1
        ins = make()
        if pend:
            so, v = pend[-1]
            ins.wait_op(so.sem, v, "sem-ge")
            e.seen[so] = v
        return ins

    def op(self, ename, fn, reads=(), writes=()):
        e = self.engs[ename]
        ins = self._emit(e, self._need(reads, writes), lambda: fn(e.eng))
        e.so.count += 1
        ins.then_inc(e.so.sem, 1)
        self._mark(e.so, e.so.count, reads, writes)
        self.ninst += 1
        return ins

    def dma(self, qname, out, in_, reads=(), writes=(), **kw):
        e = self.engs[qname]
        lst = self.dmasems[qname]
        so = lst[e.n_dma % len(lst)]
        e.n_dma += 1
        need = self._need(reads, writes)
        if so.count > 0:
            if need.get(so, 0) < so.count:
                need[so] = so.count
        ins = self._emit(e, need, lambda: e.eng.dma_start(out=out, in_=in_, **kw))
        so.count += 16
        ins.then_inc(so.sem, 16)
        self._mark(so, so.count, reads, writes)
        self.ninst += 1
        return ins

    def barrier(self):
        sos = [e.so for e in self.engs.values()] + [so for l in self.dmasems.values() for so in l]
        if hasattr(self, "ccso"):
            sos.append(self.ccso)
        for e in self.engs.values():
            need = {so: so.count for so in sos if so.count > 0}
            self._waits(e, need)

    def collective(self, kind, in_ap, out_ap, groups, reads=(), writes=()):
        e = self.engs["pool"]
        if not hasattr(self, "ccso"):
            sem = self.es0.enter_context(self.nc.semaphore("s_cc"))
            self.ccso = SyncObj(sem, 1, "cc")
        so = self.ccso
        need = self._need(reads, writes)
        if so.count > 0:
            need[so] = max(need.get(so, 0), so.count)
        self._waits(e, need)
        ins = self.nc.gpsimd.collective_compute(kind, ALU.bypass, replica_groups=groups, ins=[in_ap], outs=[out_ap])
        so.count += 1
        ins.then_inc(so.sem, 1)
        self._mark(so, so.count, reads, writes)
        self.ninst += 1

    def finish(self, toks):
        e = self.engs["sp"]
        need = self._need(list(toks), [])
        self._waits(e, need)


D = 1024
EXPM05 = 0.6065306597126334


class PB:
    def __init__(self, k):
        self.banks = [k.ps("pb%d" % i, [128, 512]) for i in range(8)]
        self.toks = [Tok("pb%d" % i) for i in range(8)]
        self.i = 0

    def get(self):
        j = self.i % 8
        self.i += 1
        return self.banks[j], self.toks[j]


def _tt(out, a, b, op):
    return lambda e: e.tensor_tensor(out=out, in0=a, in1=b, op=op)


def _ts(out, a, s1, op0, s2=None, op1=None):
    if op1 is None:
        return lambda e: e.tensor_scalar(out=out, in0=a, scalar1=s1, scalar2=None, op0=op0)
    return lambda e: e.tensor_scalar(out=out, in0=a, scalar1=s1, scalar2=s2, op0=op0, op1=op1)


def _stt(out, a, sc, b, op0, op1):
    return lambda e: e.scalar_tensor_tensor(out=out, in0=a, scalar=sc, in1=b, op0=op0, op1=op1)


def _act(out, in_, func, scale=None, bias=None):
    kw = {}
    if scale is not None:
        kw["scale"] = scale
    if bias is not None:
        kw["bias"] = bias
    return lambda e: e.activation(out=out, in_=in_, func=func, **kw)


def rsqrt(k, ap, tok):
    k.op("act", _act(ap, ap, AF.Sqrt), reads=[tok], writes=[tok])
    k.op("dve", lambda e: e.reciprocal(ap, ap), reads=[tok], writes=[tok])


def _cp(out, in_):
    return lambda e: (e.tensor_copy(out, in_) if hasattr(e, "tensor_copy") else e.copy(out, in_))


def _mm(out, lhsT, rhs, start=True, stop=True):
    return lambda e: e.matmul(out, lhsT=lhsT, rhs=rhs, start=start, stop=stop)


def _tr(out, in_, ident):
    return lambda e: e.transpose(out, in_, ident)


def _rs(out, in_):
    return lambda e: e.reduce_sum(out=out, in_=in_, axis=AX.X)


def make_consts():
    I = np.eye(128, dtype=np.float32)
    Uq = np.triu(np.ones((128, 128), np.float32))
    Lq = np.tril(np.ones((128, 128), np.float32))
    U = Uq - I
    L = Lq - I
    ones = np.ones((128, 128), np.float32)
    mAB_f = np.concatenate([-U, -Uq, U, Uq], 1)
    mAB_b = np.concatenate([-L, -Lq, L, Lq], 1)
    mC_f = np.concatenate([-L] * 4, 1)
    mC_b = np.concatenate([-U] * 4, 1)
    return np.ascontiguousarray(np.concatenate([I, Uq, Lq, ones, mAB_f, mAB_b, mC_f, mC_b], 1))


C_I, C_UQ, C_LQ, C_ONES, C_MABF, C_MABB, C_MCF, C_MCB = 0, 128, 256, 384, 512, 1024, 1536, 2048
NCONST = 2560


def emit_mixer(k, pb, nc, A, NCT, NLT, need_ctx):
    NT = NCT + NLT
    TT = NT * 128
    xin, Txc = A["xin"], A["Txc"]
    cvT, modw, modb, gain, win = A["cvT"], A["modw_m"], A["modb_m"], A["gain_m"], A["win"]
    bc64, tapsd, bc256, dupd, iupd, gupd = A["bc64"], A["taps"], A["bc256"], A["dup"], A["iup"], A["gup"]
    roped, constd = A["rope"], A["consts"]
    mto = A["mto"]
    P, BVG, GH, RB, YB, YF, YW = A["P"], A["BVG"], A["GH"], A["RB"], A["YB"], A["YF"], A["YW"]
    TMTo = A["TMTo"]

    def poff(i):
        return 1 + 128 * i if i < NCT else 3 + 128 * i

    es = ExitStack()
    with es:
        es_outer = k.es
        k.es = es
        cst = k.sb("cst", [128, NCONST]); Tc = Tok("cst")
        k.dma("sp", cst[:], constd[:, :], writes=[Tc])
        ident = cst[:, C_I:C_I + 128]
        ones128 = cst[:, C_ONES:C_ONES + 128]

        es_main = k.es
        es2 = ExitStack()
        k.es = es2
        cv = k.sb("cv", [128, 8, 2]); Tcv = Tok()
        k.dma("sp", cv[:], cvT.rearrange("(kc p) two -> p kc two", p=128), writes=[Tcv])
        scv = k.sb("scv", [128, 8, 2]); Tscv = Tok()
        k.op("act", _act(scv[:], cv[:], AF.Silu), reads=[Tcv], writes=[Tscv])
        mwt = k.sb("mwt", [128, 8, 512]); Tmw = Tok()
        mod = k.sb("mod", [128, 16, 2]); Tmod = Tok()
        mbt = k.sb("mbt", [128, 16]); Tmb = Tok()
        k.dma("sp", mbt[:], modb[:, :], writes=[Tmb])
        gt = k.sb("gt", [128, 8]); Tg = Tok()
        k.dma("sp", gt[:], gain[:, :], writes=[Tg])
        for cb in range(4):
            k.dma("sp", mwt[:], modw[:, cb * 512:(cb + 1) * 512].rearrange("(kc p) n -> p kc n", p=128), writes=[Tmw])
            bank, Tb = pb.get()
            for j in range(4):
                for kc in range(8):
                    k.op("pe", _mm(bank[:, j * 2:j * 2 + 2], mwt[:, kc, j * 128:(j + 1) * 128], scv[:, kc, :], kc == 0, kc == 7),
                         reads=[Tmw, Tscv], writes=[Tb])
            k.op("dve", _tt(mod[:, cb * 4:(cb + 1) * 4, :], bank[:, 0:8].rearrange("p (a b) -> p a b", a=4),
                            mbt[:, cb * 4:(cb + 1) * 4].unsqueeze(2).broadcast_to([128, 4, 2]), ALU.add),
                 reads=[Tb, Tmb], writes=[Tmod])
        amod = k.sb("amod", [128, 8, 2]); Tam = Tok()
        k.op("dve", _ts(amod[:], mod[:, 8:16, :], 1.0, ALU.add), reads=[Tmod], writes=[Tam])
        k.op("dve", _tt(amod[:], amod[:], gt[:, :].unsqueeze(2).broadcast_to([128, 8, 2]), ALU.mult), reads=[Tam, Tg], writes=[Tam])

        Wb = k.sb("Wb", [128, 8, 1536], BF16); TWb = Tok()
        wraw = k.sb("wraw", [128, 1536]); Twr = Tok()
        biasrow = k.sb("biasrow", [1, 1536]); Tbr = Tok()
        biasbc = k.sb("biasbc", [128, 1536]); Tbb = Tok()

        def prep_weights(which):
            banks = [pb.get() for _ in range(3)]
            for kc in range(8):
                k.dma("sp", wraw[:], win[kc * 128:(kc + 1) * 128, :], writes=[Twr])
                k.op("act", _act(Wb[:, kc, :], wraw[:], AF.Copy, scale=amod[:, kc, which:which + 1]), reads=[Twr, Tam], writes=[TWb])
                for g in range(3):
                    k.op("pe", _mm(banks[g][0][0:1, :], mod[:, kc, which:which + 1], wraw[:, g * 512:(g + 1) * 512], kc == 0, kc == 7),
                         reads=[Tmod, Twr], writes=[banks[g][1]])
            for g in range(3):
                k.op("dve", _cp(biasrow[0:1, g * 512:(g + 1) * 512], banks[g][0][0:1, :]), reads=[banks[g][1]], writes=[Tbr])
            for g in range(3):
                bank, Tb = pb.get()
                k.op("pe", _mm(bank[:, :], cst[0:1, C_ONES:C_ONES + 128], biasrow[0:1, g * 512:(g + 1) * 512]), reads=[Tc, Tbr], writes=[Tb])
                k.op("dve", _cp(biasbc[:, g * 512:(g + 1) * 512], bank[:, :]), reads=[Tb], writes=[Tbb])

        QT = k.sb("QT", [64, 4, TT], BF16); TQ = [Tok() for _ in range(NT)]
        KTt = k.sb("KTt", [64, TT], BF16); TK = [Tok() for _ in range(NT)]
        Vt = k.sb("Vt", [128, NT, 65], BF16); TV = [Tok() for _ in range(NT)]
        Tv1 = Tok()
        k.op("dve", lambda e: e.memset(Vt[:, :, 64:65], 1.0), writes=[Tv1])
        g5 = k.sb("g5", [128, 5, 64]); Tg5 = Tok()
        k.dma("sp", g5[:], bc64[:, :, :], writes=[Tg5])
        X2 = [k.sb("xt%d" % i, [128, D]) for i in range(2)]; TX = [Tok(), Tok()]
        sqj = k.sb("sqj", [128, D]); Tsq = Tok()
        xn = k.sb("xn", [128, D]); Txn = Tok()
        ss = k.sb("ss", [128, 1]); Tss = Tok()
        xT = k.sb("xT", [128, 8, 128], BF16); TxT = Tok()
        pt = k.sb("pt", [128, 1536]); Tp = Tok()
        rp = k.sb("rp", [128, 2, 64]); Trp = Tok()
        ss5 = k.sb("ss5", [128, 5]); Ts5 = Tok()
        qn = k.sb("qn", [128, 5, 64]); Tqn = Tok()
        qa = k.sb("qa", [128, 5, 64]); Tqa = Tok()
        qb = k.sb("qb", [128, 5, 64]); Tqb = Tok()
        zrow = k.sb("zrow", [1, 1152]); Tzr = Tok()
        k.op("dve", lambda e: e.memset(zrow[:], 0.0), writes=[Tzr])
        TPz = Tok()
        for r_ in (0, NCT * 128 + 1, NCT * 128 + 2, TT + 3):
            k.dma("sp", P[r_:r_ + 1, :], zrow[0:1, :], reads=[Tzr], writes=[TPz])
        TP = [Tok() for _ in range(NT)]

        def phaseA(i):
            which = 1 if i < NCT else 0
            xt = X2[i % 2]; Tx = TX[i % 2]
            k.dma("sp", xt[:], xin(i), reads=[Txc], writes=[Tx])
            k.dma("sp", rp[:], roped[i * 128:(i + 1) * 128, :, :], writes=[Trp])
            k.op("act", _act(sqj[:], xt[:], AF.Square), reads=[Tx], writes=[Tsq])
            k.op("dve", _rs(ss[:], sqj[:]), reads=[Tsq], writes=[Tss])
            k.op("dve", _ts(ss[:], ss[:], 1.0 / D, ALU.mult, 1e-6, ALU.add), reads=[Tss], writes=[Tss])
            rsqrt(k, ss[:], Tss)
            k.op("act", _act(xn[:], xt[:], AF.Copy, scale=ss[:, 0:1]), reads=[Tx, Tss], writes=[Txn])
            for half in range(2):
                bank, Tb = pb.get()
                for j in range(4):
                    c0 = (half * 4 + j) * 128
                    k.op("pe", _tr(bank[:, j * 128:(j + 1) * 128], xn[:, c0:c0 + 128], ident), reads=[Txn, Tc], writes=[Tb])
                k.op("dve" if half == 0 else "act", _cp(xT[:, half * 4:(half + 1) * 4, :], bank[:, :].rearrange("p (a b) -> p a b", a=4)),
                     reads=[Tb], writes=[TxT])
            for g in range(3):
                bank, Tb = pb.get()
                for kc in range(8):
                    k.op("pe", _mm(bank[:, :], xT[:, kc, :], Wb[:, kc, g * 512:(g + 1) * 512], kc == 0, kc == 7), reads=[TxT, TWb], writes=[Tb])
                k.op("dve", _tt(pt[:, g * 512:(g + 1) * 512], bank[:, :], biasbc[:, g * 512:(g + 1) * 512], ALU.add), reads=[Tb, Tbb], writes=[Tp])
            k.dma("sp", P[poff(i):poff(i) + 128, :], pt[:, 384:1536], reads=[Tp], writes=[TP[i]])
            q5 = pt[:, 0:320].rearrange("p (h d) -> p h d", h=5)
            k.op("dve", _tt(qa[:], q5, q5, ALU.mult), reads=[Tp], writes=[Tqa])
            k.op("dve", _rs(ss5[:], qa[:]), reads=[Tqa], writes=[Ts5])
            k.op("dve", _ts(ss5[:], ss5[:], 1.0 / 64, ALU.mult, 1e-6, ALU.add), reads=[Ts5], writes=[Ts5])
            rsqrt(k, ss5[:], Ts5)
            k.op("dve", _tt(qn[:], q5, ss5[:, :].unsqueeze(2).broadcast_to([128, 5, 64]), ALU.mult), reads=[Tp, Ts5], writes=[Tqn])
            k.op("dve", _tt(qn[:], qn[:], g5[:], ALU.mult), reads=[Tqn, Tg5], writes=[Tqn])
            k.op("dve", _tt(qa[:], qn[:], rp[:, 0, :].unsqueeze(1).broadcast_to([128, 5, 64]), ALU.mult), reads=[Tqn, Trp], writes=[Tqa])
            qn6 = qn[:].rearrange("p h (a b f) -> p h a b f", a=2, b=2)
            qb6 = qb[:].rearrange("p h (a b f) -> p h a b f", a=2, b=2)
            rs6 = rp[:, 1, :].rearrange("p (a b f) -> p a b f", a=2, b=2)
            for hb in range(2):
                k.op("dve", _tt(qb6[:, :, :, hb, :], qn6[:, :, :, 1 - hb, :],
                                rs6[:, :, hb, :].unsqueeze(1).broadcast_to([128, 5, 2, 16]), ALU.mult),
                     reads=[Tqn, Trp], writes=[Tqb])
            k.op("dve", _tt(qa[:], qa[:], qb[:], ALU.add), reads=[Tqa, Tqb], writes=[Tqa])
            bank, Tb = pb.get()
            for h in range(4):
                k.op("pe", _tr(bank[0:64, h * 128:(h + 1) * 128], qa[:, h, :], ident), reads=[Tqa, Tc], writes=[Tb])
            k.op("act", _cp(QT[:, :, i * 128:(i + 1) * 128], bank[0:64, :].rearrange("p (h t) -> p h t", h=4)), reads=[Tb], writes=[TQ[i]])
            bank, Tb = pb.get()
            k.op("pe", _tr(bank[0:64, 0:128], qa[:, 4, :], ident), reads=[Tqa, Tc], writes=[Tb])
            k.op("act", _cp(KTt[:, i * 128:(i + 1) * 128], bank[0:64, 0:128]), reads=[Tb], writes=[TK[i]])
            k.op("dve", _cp(Vt[:, i, 0:64], pt[:, 320:384]), reads=[Tp, Tv1], writes=[TV[i]])

        if NCT > 0:
            prep_weights(1)
            for i in range(NCT):
                phaseA(i)
        prep_weights(0)
        for i in range(NCT, NT):
            phaseA(i)

        PTs = [k.sb("PT%d" % i, [128, 512], BF16) for i in range(2)]; TPT = [Tok(), Tok()]
        Osb = k.sb("Osb", [65, 512]); TO = Tok()
        At = k.sb("At", [64, 512]); TAt = Tok()
        Tout = TMTo
        qtiles = list(range(NCT, NT)) + (list(range(NCT)) if need_ctx else [])
        for qi in qtiles:
            keys = list(range(NT)) if qi >= NCT else list(range(NCT))
            po, Tpo = pb.get()
            def issue_S(n):
                kt_ = keys[n]
                ps, Tps = pb.get()
                if ps is po:
                    ps, Tps = pb.get()
                k.op("pe", _mm(ps[:, :], KTt[:, kt_ * 128:(kt_ + 1) * 128], QT[:, :, qi * 128:(qi + 1) * 128]), reads=[TK[kt_], TQ[qi]], writes=[Tps])
                return ps, Tps
            cur = issue_S(0)
            for n, kt in enumerate(keys):
                nxt = issue_S(n + 1) if n + 1 < len(keys) else None
                ps, Tps = cur
                k.op("act", _act(PTs[n % 2][:], ps[:, :], AF.Exp, scale=0.125), reads=[Tps], writes=[TPT[n % 2]])
                k.op("pe", _mm(po[0:65, :], Vt[:, kt, :], PTs[n % 2][:], n == 0, n == len(keys) - 1), reads=[TV[kt], TPT[n % 2]], writes=[Tpo])
                cur = nxt
            k.op("dve", _cp(Osb[0:65, :], po[0:65, :]), reads=[Tpo], writes=[TO])
            k.op("dve", lambda e: e.reciprocal(Osb[64:65, :], Osb[64:65, :]), reads=[TO], writes=[TO])
            bank, Tb = pb.get()
            k.op("pe", _mm(bank[0:64, :], cst[64:65, C_ONES:C_ONES + 64], Osb[64:65, :]), reads=[Tc, TO], writes=[Tb])
            k.op("dve", _tt(At[:, :], Osb[0:64, :], bank[0:64, :], ALU.mult), reads=[TO, Tb], writes=[TAt])
            k.dma("sp", mto(qi)[0:256, :].rearrange("(h d) t -> d h t", h=4), At[:, :].rearrange("p (h t) -> p h t", h=4), reads=[TAt], writes=[Tout])

        k.barrier()
        es2.close()
        k.es = es_main
        build_rwkv(k, pb, nc, locals())
        k.barrier()
        k.es = es_outer
        print("mixer ninst", k.ninst)


def build_rwkv(k, pb, nc, L):
    cst, Tc, ident = L["cst"], L["Tc"], L["ident"]
    P, TP, TPz, poff = L["P"], L["TP"], L["TPz"], L["poff"]
    BVG, GH, RB, YB, YF, YW = L["BVG"], L["GH"], L["RB"], L["YB"], L["YF"], L["YW"]
    mto, TMTo = L["mto"], L["TMTo"]
    NCT, NT, need_ctx = L["NCT"], L["NT"], L["need_ctx"]
    ones128 = cst[:, C_ONES:C_ONES + 128]

    taps = k.sb("tapsb", [128, 3, 1152]); Ttp = Tok()
    k.dma("sp", taps[:], L["tapsd"][:, :, :], writes=[Ttp])
    b256 = k.sb("b256", [128, 5, 256]); Tb2 = Tok()
    k.dma("sp", b256[:], L["bc256"][:, :, :], writes=[Tb2])
    kk_bc, ka_bc, rk_bc, lnw_bc, lnb_bc = (b256[:, j, :] for j in range(5))
    omka = k.sb("omka", [128, 256]); Tom = Tok()
    k.op("dve", _ts(omka[:], ka_bc, -1.0, ALU.mult, 1.0, ALU.add), reads=[Tb2], writes=[Tom])
    dupw = k.sb("dupw", [65, 2, 256]); iupw = k.sb("iupw", [65, 2, 256]); gupw = k.sb("gupw", [128, 256]); Tw = Tok()
    k.dma("sp", dupw[:], L["dupd"][:, :, :], writes=[Tw])
    k.dma("sp", iupw[:], L["iupd"][:, :, :], writes=[Tw])
    k.dma("sp", gupw[:], L["gupd"][:, :], writes=[Tw])

    Pm = k.sb("Pm", [128, 1152]); P0 = k.sb("P0", [128, 1152]); Pp = k.sb("Pp", [128, 1152]); TPl = Tok()
    cols = k.sb("cols", [128, 1152]); Tco = Tok()
    wdt = k.sb("wdt", [128, 128]); sg = k.sb("sg", [128, 128]); Twd = Tok()
    LT = k.sb("LT", [65, 4, 128]); TLT = Tok()
    TLo = Tok()
    k.op("dve", lambda e: e.memset(LT[64:65, :, :], 1.0), writes=[TLo])
    sgT = k.sb("sgT", [128, 128]); TsgT = Tok()
    lw = k.sb("lw", [128, 512]); Tlw = Tok()
    aa = k.sb("aa", [128, 512]); Taa = Tok()
    bvg = k.sb("bvg", [128, 512]); Tbvg = Tok()
    kkt = k.sb("kkt", [128, 256]); Tkk = Tok()
    t256 = k.sb("t256", [128, 256]); Tt2 = Tok()
    s4 = k.sb("s4", [128, 4]); Ts4 = Tok()
    kd = k.sb("kd", [128, 512]); Tkd = Tok()
    bb = k.sb("bb", [128, 512]); Tbb_ = Tok()
    ecp = k.sb("ecp", [128, 512]); ecm = k.sb("ecm", [128, 512]); iw = k.sb("iw", [128, 512]); etot = k.sb("etot", [128, 512])
    Te = Tok()
    dcol = k.sb("dcol", [64, 8]); Tdc = Tok()
    al = k.sb("al", [128, 512]); be = k.sb("be", [128, 512]); ka_ = k.sb("ka_", [128, 512]); rho = k.sb("rho", [128, 512])
    bepn = k.sb("bepn", [128, 512]); kap = k.sb("kap", [128, 512]); Tf = Tok()
    ART = k.sb("ART", [128, 4, 2, 128]); BTt = k.sb("BTt", [128, 4, 128]); KTk = k.sb("KTk", [128, 4, 128]); TT_ = Tok()
    XM = k.sb("XM", [128, 8, 512]); TXM = [Tok() for _ in range(8)]
    XT0 = k.sb("XT0", [128, 8, 128]); TXT0h = [Tok(), Tok()]
    XS = [k.sb("XS%d" % i, [128, 8, 128]) for i in range(2)]; TXSh = [[Tok(), Tok()], [Tok(), Tok()]]
    XTS = [k.sb("XTS%d" % i, [128, 8, 128]) for i in range(2)]; TXTSh = [[Tok(), Tok()], [Tok(), Tok()]]
    Z = k.sb("Z", [128, 8, 128]); TZh = [Tok(), Tok()]
    Rbs = k.sb("Rbs", [64, 1024]); TRbs = Tok()
    Ybs = k.sb("Ybs", [128, 512]); TYbs = Tok()
    GHs = k.sb("GHs", [64, 1024]); TGHs = Tok()
    TBVG = [Tok() for _ in range(NT)]
    TGH = [Tok() for _ in range(NT)]; TRB = [Tok() for _ in range(NT)]; TYB = [Tok() for _ in range(NT)]

    def bc_d(ap256):
        return ap256.unsqueeze(1).broadcast_to([128, 2, 256])

    def v3(ap512):
        return ap512.rearrange("p (d c) -> p d c", d=2)

    def h4(ap256):
        return ap256.rearrange("p (h c) -> p h c", h=4)

    def phaseB(i):
        o = poff(i)
        rd = [TP[i], TPz] + ([TP[i - 1]] if i > 0 else []) + ([TP[i + 1]] if i + 1 < NT else [])
        k.dma("sp", Pm[:], P[o - 1:o + 127, :], reads=rd, writes=[TPl])
        k.dma("sp", P0[:], P[o:o + 128, :], reads=rd, writes=[TPl])
        k.dma("sp", Pp[:], P[o + 1:o + 129, :], reads=rd, writes=[TPl])
        k.op("dve", _tt(cols[:], Pm[:], taps[:, 0, :], ALU.mult), reads=[TPl, Ttp], writes=[Tco])
        k.op("dve", _tt(P0[:], P0[:], taps[:, 1, :], ALU.mult), reads=[TPl, Ttp], writes=[TPl])
        k.op("dve", _tt(Pp[:], Pp[:], taps[:, 2, :], ALU.mult), reads=[TPl, Ttp], writes=[TPl])
        k.op("dve", _tt(cols[:], cols[:], P0[:], ALU.add), reads=[TPl, Tco], writes=[Tco])
        k.op("dve", _tt(cols[:], cols[:], Pp[:], ALU.add), reads=[TPl, Tco], writes=[Tco])
        wd = cols[:, 0:128]; r_ = cols[:, 128:384]; kr = cols[:, 384:640]; vr = cols[:, 640:896]
        ad = cols[:, 896:1024]; gd = cols[:, 1024:1152]
        k.op("act", _act(wdt[:], wd, AF.Tanh), reads=[Tco], writes=[Twd])
        k.op("act", _act(sg[:], gd, AF.Sigmoid), reads=[Tco], writes=[Twd])
        bank, Tb = pb.get()
        for d in range(2):
            k.op("pe", _tr(bank[0:64, d * 128:(d + 1) * 128], wdt[:, d * 64:(d + 1) * 64], ident), reads=[Twd, Tc], writes=[Tb])
            k.op("pe", _tr(bank[0:64, (2 + d) * 128:(3 + d) * 128], cols[:, 896 + d * 64:896 + (d + 1) * 64], ident), reads=[Tco, Tc], writes=[Tb])
        k.op("dve", _cp(LT[0:64, :, :], bank[0:64, :].rearrange("p (a t) -> p a t", a=4)), reads=[Tb, TLo], writes=[TLT])
        bank, Tb = pb.get()
        k.op("pe", _tr(bank[:, 0:128], sg[:], ident), reads=[Twd, Tc], writes=[Tb])
        k.op("act", _cp(sgT[:], bank[:, 0:128]), reads=[Tb], writes=[TsgT])
        bz, Tbz = pb.get()
        bi, Tbi = pb.get()
        bg, Tbg = pb.get()
        for d in range(2):
            k.op("pe", _mm(bz[:, d * 256:(d + 1) * 256], LT[0:65, d, :], dupw[0:65, d, :]), reads=[TLT, Tw], writes=[Tbz])
            k.op("pe", _mm(bi[:, d * 256:(d + 1) * 256], LT[0:65, 2 + d, :], iupw[0:65, d, :]), reads=[TLT, Tw], writes=[Tbi])
        k.op("pe", _mm(bg[:, 0:256], sgT[:], gupw[:]), reads=[TsgT, Tw], writes=[Tbg])
        k.op("act", _act(lw[:], bz[:, :], AF.Sigmoid), reads=[Tbz], writes=[Tlw])
        k.op("dve", _ts(lw[:], lw[:], -EXPM05, ALU.mult), reads=[Tlw], writes=[Tlw])
        k.op("act", _act(aa[:], bi[:, :], AF.Sigmoid), reads=[Tbi], writes=[Taa])
        k.op("act", _cp(bvg[:, 256:512], bg[:, 0:256]), reads=[Tbg], writes=[Tbvg])
        k.op("dve", _tt(kkt[:], kr, kk_bc, ALU.mult), reads=[Tco, Tb2], writes=[Tkk])
        k.op("dve", _tt(t256[:], kkt[:], kkt[:], ALU.mult), reads=[Tkk], writes=[Tt2])
        k.op("dve", _rs(s4[:], h4(t256[:])), reads=[Tt2], writes=[Ts4])
        k.op("dve", _ts(s4[:], s4[:], 1e-12, ALU.add), reads=[Ts4], writes=[Ts4])
        rsqrt(k, s4[:], Ts4)
        k.op("dve", _tt(h4(kkt[:]), h4(kkt[:]), s4[:, :].unsqueeze(2).broadcast_to([128, 4, 64]), ALU.mult), reads=[Tkk, Ts4], writes=[Tkk])
        k.op("dve", _tt(v3(kd[:]), v3(aa[:]), bc_d(ka_bc), ALU.mult), reads=[Taa, Tb2], writes=[Tkd])
        k.op("dve", _tt(v3(kd[:]), v3(kd[:]), bc_d(omka[:]), ALU.add), reads=[Tkd, Tom], writes=[Tkd])
        k.op("dve", _tt(v3(kd[:]), v3(kd[:]), bc_d(kr), ALU.mult), reads=[Tkd, Tco], writes=[Tkd])
        k.op("dve", _tt(v3(bb[:]), v3(aa[:]), bc_d(kkt[:]), ALU.mult), reads=[Taa, Tkk], writes=[Tbb_])
        k.op("dve", _tt(t256[:], r_, kr, ALU.mult), reads=[Tco, Ts4], writes=[Tt2])
        k.op("dve", _tt(t256[:], t256[:], rk_bc, ALU.mult), reads=[Tt2, Tb2], writes=[Tt2])
        k.op("dve", _rs(s4[:], h4(t256[:])), reads=[Tt2, Tkk], writes=[Ts4])
        k.op("dve", _tt(h4(bvg[:, 0:256]), h4(vr), s4[:, :].unsqueeze(2).broadcast_to([128, 4, 64]), ALU.mult), reads=[Tco, Ts4], writes=[Tbvg])
        k.dma("sp", BVG[i * 128:(i + 1) * 128, :], bvg[:], reads=[Tbvg], writes=[TBVG[i]])
        bc_, Tbc = pb.get()
        bt_, Tbt = pb.get()
        bd_, Tbd = pb.get()
        k.op("pe", _mm(bc_[:, 0:256], cst[:, C_UQ:C_UQ + 128], lw[:, 0:256]), reads=[Tc, Tlw], writes=[Tbc])
        k.op("pe", _mm(bc_[:, 256:512], cst[:, C_LQ:C_LQ + 128], lw[:, 256:512]), reads=[Tc, Tlw], writes=[Tbc])
        k.op("pe", _mm(bt_[:, :], ones128, lw[:, :]), reads=[Tc, Tlw], writes=[Tbt])
        for u in range(8):
            k.op("pe", _mm(bd_[0:64, u:u + 1], lw[:, u * 64:(u + 1) * 64], cst[:, C_ONES:C_ONES + 1]), reads=[Tc, Tlw], writes=[Tbd])
        k.op("act", _act(ecp[:], bc_[:, :], AF.Exp), reads=[Tbc], writes=[Te])
        k.op("act", _act(ecm[:], bc_[:, :], AF.Exp, scale=-1.0), reads=[Tbc], writes=[Te])
        k.op("act", _act(iw[:], lw[:], AF.Exp, scale=-1.0), reads=[Tlw], writes=[Te])
        k.op("act", _act(etot[:], bt_[:, :], AF.Exp), reads=[Tbt], writes=[Te])
        k.op("act", _act(dcol[:, :], bd_[0:64, 0:8], AF.Exp), reads=[Tbd], writes=[Tdc])
        k.op("dve", _tt(iw[:], iw[:], ecp[:], ALU.mult), reads=[Te], writes=[Te])
        k.op("dve", _tt(etot[:], etot[:], ecm[:], ALU.mult), reads=[Te], writes=[Te])
        k.op("dve", _tt(v3(al[:]), v3(iw[:]), bc_d(kkt[:]), ALU.mult), reads=[Te, Tkk], writes=[Tf])
        k.op("dve", _tt(be[:], bb[:], ecm[:], ALU.mult), reads=[Te, Tbb_], writes=[Tf])
        k.op("dve", _tt(ka_[:], kd[:], ecm[:], ALU.mult), reads=[Te, Tkd], writes=[Tf])
        k.op("dve", _tt(v3(rho[:]), v3(ecp[:]), bc_d(r_), ALU.mult), reads=[Te, Tco], writes=[Tf])
        k.op("dve", _stt(bepn[:], bb[:], -1.0, etot[:], ALU.mult, ALU.mult), reads=[Te, Tbb_], writes=[Tf])
        k.op("dve", _tt(kap[:], kd[:], etot[:], ALU.mult), reads=[Te, Tkd], writes=[Tf])
        for src, dst in ((al, lambda: ART[:, :, 0, :]), (rho, lambda: ART[:, :, 1, :]), (be, lambda: BTt[:, :, :]), (ka_, lambda: KTk[:, :, :])):
            bank, Tb = pb.get()
            for pr in range(4):
                k.op("pe", _tr(bank[:, pr * 128:(pr + 1) * 128], src[:, pr * 128:(pr + 1) * 128], ident), reads=[Tf, Tc], writes=[Tb])
            k.op("act", _cp(dst(), bank[:, :].rearrange("p (a t) -> p a t", a=4)), reads=[Tb], writes=[TT_])
        for half in range(2):
            mab = cst[:, (C_MABF if half == 0 else C_MABB):(C_MABF if half == 0 else C_MABB) + 512]
            mc = cst[:, (C_MCF if half == 0 else C_MCB):(C_MCF if half == 0 else C_MCB) + 512]
            bC, TbC = pb.get()
            for uu in range(4):
                u = half * 4 + uu
                pr = u // 2; rws = slice((u % 2) * 64, (u % 2) * 64 + 64)
                bA, TbA = pb.get()
                if bA is bC:
                    bA, TbA = pb.get()
                arr = ART[rws, pr, :, :].rearrange("p a t -> p (a t)")
                k.op("pe", _mm(bA[:, 0:256], BTt[rws, pr, :], arr), reads=[TT_], writes=[TbA])
                k.op("pe", _mm(bA[:, 256:512], KTk[rws, pr, :], arr), reads=[TT_], writes=[TbA])
                k.op("pe", _mm(bC[:, uu * 128:(uu + 1) * 128], ART[rws, pr, 0, :], BTt[rws, pr, :]), reads=[TT_], writes=[TbC])
                k.op("dve", _tt(XM[:, u, :], bA[:, :], mab, ALU.mult), reads=[TbA, Tc], writes=[TXM[u]])
            k.op("dve", _tt(XT0[:, half * 4:(half + 1) * 4, :], bC[:, :].rearrange("p (a t) -> p a t", a=4),
                            mc.rearrange("p (a t) -> p a t", a=4), ALU.mult), reads=[TbC, Tc], writes=[TXT0h[half]])
        bank, Tb = pb.get()
        for u in range(8):
            h = u % 4
            k.op("pe", _mm(bank[:, u * 64:(u + 1) * 64], XM[:, u, 256:384], cols[:, 640 + h * 64:640 + (h + 1) * 64]), reads=[TXM[u], Tco], writes=[Tb])
        k.op("dve", _cp(Z[:, :, 64:128], bank[:, :].rearrange("p (u c) -> p u c", u=8)), reads=[Tb], writes=TZh)
        k.op("act", _cp(Z[:, :, 0:64], al[:].rearrange("p (u c) -> p u c", u=8)), reads=[Tf] + TZh, writes=TZh)
        NR = 7
        for m in range(NR):
            pend = []
            for hb in range(2):
                if m == 0:
                    Xc = lambda u: XM[:, u, 0:128]
                    XTc = lambda u: XT0[:, u, :]
                    rX = lambda u: [TXM[u]]
                    rXT = lambda u, hb=hb: [TXT0h[hb]]
                else:
                    Xc = (lambda mm_: (lambda u: XS[mm_ % 2][:, u, :]))(m)
                    XTc = (lambda mm_: (lambda u: XTS[mm_ % 2][:, u, :]))(m)
                    rX = (lambda mm_, hb=hb: (lambda u: [TXSh[mm_ % 2][hb]]))(m)
                    rXT = (lambda mm_, hb=hb: (lambda u: [TXTSh[mm_ % 2][hb]]))(m)
                bk, Tbk_ = pb.get()
                for uu in range(4):
                    u = hb * 4 + uu
                    k.op("pe", _mm(bk[:, uu * 128:(uu + 1) * 128], Xc(u), Z[:, u, :]), reads=rX(u) + [TZh[hb]], writes=[Tbk_])
                bx = bxt = None
                if m < NR - 1:
                    bx = pb.get(); bxt = pb.get()
                    for uu in range(4):
                        u = hb * 4 + uu
                        sl = slice(uu * 128, (uu + 1) * 128)
                        k.op("pe", _mm(bx[0][:, sl], XTc(u), Xc(u)), reads=rX(u) + rXT(u), writes=[bx[1]])
                        k.op("pe", _mm(bxt[0][:, sl], Xc(u), XTc(u)), reads=rX(u) + rXT(u), writes=[bxt[1]])
                pend.append((hb, bk, Tbk_, bx, bxt))
            for hb, bk, Tbk_, bx, bxt in pend:
                hs_ = slice(hb * 4, (hb + 1) * 4)
                k.op("dve", _tt(Z[:, hs_, :], Z[:, hs_, :], bk[:, :].rearrange("p (a t) -> p a t", a=4), ALU.add), reads=[Tbk_, TZh[hb]], writes=[TZh[hb]])
                if m < NR - 1:
                    k.op("act", _cp(XS[(m + 1) % 2][:, hs_, :], bx[0][:, :].rearrange("p (a t) -> p a t", a=4)), reads=[bx[1]], writes=[TXSh[(m + 1) % 2][hb]])
                    k.op("dve" if hb == 0 else "act", _cp(XTS[(m + 1) % 2][:, hs_, :], bxt[0][:, :].rearrange("p (a t) -> p a t", a=4)),
                         reads=[bxt[1]], writes=[TXTSh[(m + 1) % 2][hb]])
        br = [pb.get(), pb.get()]
        by, Tby = pb.get()
        bgm, Tbgm = pb.get()
        bh, Tbh = pb.get()
        for u in range(8):
            h = u % 4
            vh = cols[:, 640 + h * 64:640 + (h + 1) * 64]
            sl = slice((u % 4) * 128, (u % 4 + 1) * 128)
            us = slice(u * 64, (u + 1) * 64)
            k.op("pe", _mm(br[u // 4][0][0:64, sl], rho[:, us], ident, True, False), reads=[Tf, Tc], writes=[br[u // 4][1]])
            k.op("pe", _mm(br[u // 4][0][0:64, sl], Z[:, u, 0:64], XM[:, u, 128:256], False, True), reads=[TZh[u // 4], TXM[u]], writes=[br[u // 4][1]])
            k.op("pe", _mm(by[:, us], XM[:, u, 384:512], vh, True, False), reads=[TXM[u], Tco], writes=[Tby])
            k.op("pe", _mm(by[:, us], XM[:, u, 128:256], Z[:, u, 64:128], False, True), reads=[TXM[u], TZh[u // 4]], writes=[Tby])
            k.op("pe", _mm(bgm[0:64, us], Z[:, u, 0:64], bepn[:, us]), reads=[TZh[u // 4], Tf], writes=[Tbgm])
            k.op("pe", _mm(bh[0:64, us], kap[:, us], vh, True, False), reads=[Tf, Tco], writes=[Tbh])
            k.op("pe", _mm(bh[0:64, us], bepn[:, us], Z[:, u, 64:128], False, True), reads=[Tf, TZh[u // 4]], writes=[Tbh])
        for hb in range(2):
            k.op("act", _cp(Rbs[:, hb * 512:(hb + 1) * 512], br[hb][0][0:64, :]), reads=[br[hb][1]], writes=[TRbs])
        k.op("dve", _cp(Ybs[:], by[:, :]), reads=[Tby], writes=[TYbs])
        for u in range(8):
            us = slice(u * 64, (u + 1) * 64)
            k.op("dve", _stt(GHs[:, us], cst[0:64, C_I:C_I + 64], dcol[:, u:u + 1], bgm[0:64, us], ALU.mult, ALU.add), reads=[Tc, Tdc, Tbgm], writes=[TGHs])
        k.op("act", _cp(GHs[:, 512:1024], bh[0:64, :]), reads=[Tbh], writes=[TGHs])
        k.dma("sp", RB[i, :, :], Rbs[:], reads=[TRbs], writes=[TRB[i]])
        k.dma("sp", YB[i, :, :], Ybs[:], reads=[TYbs], writes=[TYB[i]])
        k.dma("sp", GH[i, :, :], GHs[:], reads=[TGHs], writes=[TGH[i]])

    for i in range(NT):
        phaseB(i)

    ST = k.sb("ST", [64, 512]); TST = Tok()
    k.op("dve", lambda e: e.memset(ST[:], 0.0), writes=[TST])
    GHc = [k.sb("GHc%d" % i, [64, 1024]) for i in range(2)]; TGc = [Tok(), Tok()]
    RBc = [k.sb("RBc%d" % i, [64, 1024]) for i in range(2)]; TRc = [Tok(), Tok()]
    yo = [k.sb("yo%d" % i, [128, 512]) for i in range(2)]; Tyo = [Tok(), Tok()]
    TYF = [Tok() for _ in range(NT)]; TYW = [Tok() for _ in range(NT)]
    order_f = list(range(NT))
    order_b = list(range(NCT - 1, -1, -1)) + list(range(NT - 1, NCT - 1, -1))
    for s in range(NT):
        tf, tb = order_f[s], order_b[s]
        g, Tg_ = GHc[s % 2], TGc[s % 2]
        rb, Trb_ = RBc[s % 2], TRc[s % 2]
        gv = g[:].rearrange("p (a c) -> p a c", a=2)
        k.dma("pool", gv[:, :, 0:256], GH[tf, :, :].rearrange("p (a c) -> p a c", a=2)[:, :, 0:256], reads=[TGH[tf]], writes=[Tg_])
        k.dma("pool", gv[:, :, 256:512], GH[tb, :, :].rearrange("p (a c) -> p a c", a=2)[:, :, 256:512], reads=[TGH[tb]], writes=[Tg_])
        k.dma("pool", rb[:, 0:512], RB[tf, :, 0:512], reads=[TRB[tf]], writes=[Trb_])
        k.dma("pool", rb[:, 512:1024], RB[tb, :, 512:1024], reads=[TRB[tb]], writes=[Trb_])
        by, Tby = pb.get()
        bs, Tbs = pb.get()
        for u in range(8):
            us = slice(u * 64, (u + 1) * 64)
            k.op("pe", _mm(by[:, us], rb[:, u * 128:(u + 1) * 128], ST[:, us]), reads=[Trb_, TST], writes=[Tby])
        for u in range(8):
            us = slice(u * 64, (u + 1) * 64)
            k.op("pe", _mm(bs[0:64, us], g[:, us], ST[:, us]), reads=[Tg_, TST], writes=[Tbs])
        k.op("act", _cp(yo[s % 2][:], by[:, :]), reads=[Tby], writes=[Tyo[s % 2]])
        k.op("dve", _tt(ST[:], bs[0:64, :], g[:, 512:1024], ALU.add), reads=[Tbs, Tg_, TST], writes=[TST])
        k.dma("sp", YF[tf, :, :], yo[s % 2][:, 0:256], reads=[Tyo[s % 2]], writes=[TYF[tf]])
        k.dma("sp", YW[tb, :, :], yo[s % 2][:, 256:512], reads=[Tyo[s % 2]], writes=[TYW[tb]])

    yf = k.sb("yf", [128, 256]); yw = k.sb("yw", [128, 256]); ybl = k.sb("ybl", [128, 512]); bvl = k.sb("bvl", [128, 512]); TDl = Tok()
    yy = k.sb("yy", [128, 256]); Tyy = Tok()
    m4 = k.sb("m4", [128, 4]); Tm4 = Tok()
    ywT = k.sb("ywT", [128, 2, 128]); TywT = Tok()
    for i in (range(NT) if need_ctx else range(NCT, NT)):
        k.dma("sp", yf[:], YF[i, :, :], reads=[TYF[i]], writes=[TDl])
        k.dma("sp", yw[:], YW[i, :, :], reads=[TYW[i]], writes=[TDl])
        k.dma("sp", ybl[:], YB[i, :, :], reads=[TYB[i]], writes=[TDl])
        k.dma("sp", bvl[:], BVG[i * 128:(i + 1) * 128, :], reads=[TBVG[i]], writes=[TDl])
        k.op("dve", _tt(yy[:], yf[:], yw[:], ALU.add), reads=[TDl], writes=[Tyy])
        k.op("dve", _tt(yy[:], yy[:], ybl[:, 0:256], ALU.add), reads=[TDl, Tyy], writes=[Tyy])
        k.op("dve", _tt(yy[:], yy[:], ybl[:, 256:512], ALU.add), reads=[TDl, Tyy], writes=[Tyy])
        k.op("dve", _rs(m4[:], h4(yy[:])), reads=[Tyy], writes=[Tm4])
        k.op("dve", _ts(m4[:], m4[:], 1.0 / 64, ALU.mult), reads=[Tm4], writes=[Tm4])
        k.op("dve", _tt(h4(yy[:]), h4(yy[:]), m4[:, :].unsqueeze(2).broadcast_to([128, 4, 64]), ALU.subtract), reads=[Tyy, Tm4], writes=[Tyy])
        k.op("dve", _tt(yf[:], yy[:], yy[:], ALU.mult), reads=[Tyy, TDl], writes=[TDl])
        k.op("dve", _rs(m4[:], h4(yf[:])), reads=[TDl, Tyy], writes=[Tm4])
        k.op("dve", _ts(m4[:], m4[:], 1.0 / 64, ALU.mult, 64e-5, ALU.add), reads=[Tm4], writes=[Tm4])
        rsqrt(k, m4[:], Tm4)
        k.op("dve", _tt(h4(yy[:]), h4(yy[:]), m4[:, :].unsqueeze(2).broadcast_to([128, 4, 64]), ALU.mult), reads=[Tyy, Tm4], writes=[Tyy])
        k.op("dve", _tt(yy[:], yy[:], lnw_bc, ALU.mult), reads=[Tyy, Tb2], writes=[Tyy])
        k.op("dve", _tt(yy[:], yy[:], lnb_bc, ALU.add), reads=[Tyy, Tb2], writes=[Tyy])
        k.op("dve", _tt(yy[:], yy[:], bvl[:, 0:256], ALU.add), reads=[Tyy, TDl], writes=[Tyy])
        k.op("dve", _tt(yw[:], yy[:], bvl[:, 256:512], ALU.mult), reads=[Tyy, TDl], writes=[TDl])
        bank, Tb = pb.get()
        for j in range(2):
            k.op("pe", _tr(bank[:, j * 128:(j + 1) * 128], yw[:, j * 128:(j + 1) * 128], ident), reads=[TDl, Tc], writes=[Tb])
        k.op("act", _cp(ywT[:], bank[:, 0:256].rearrange("p (a t) -> p a t", a=2)), reads=[Tb], writes=[TywT])
        k.dma("sp", mto(i)[256:512, :].rearrange("(a p) t -> p a t", p=128), ywT[:], reads=[TywT], writes=[TMTo])


def _bc(v, n=128):
    return np.ascontiguousarray(np.broadcast_to(np.asarray(v, np.float32)[None], (n,) + tuple(np.shape(v))))


def rope_tables(n_ctx, n_lat, grid_w=64):
    t = np.arange(n_lat)
    row = (t // grid_w).astype(np.float32)
    col = (t % grid_w).astype(np.float32)
    inv = (np.float32(10000.0) ** (-np.arange(16, dtype=np.float32) * np.float32(2.0) / np.float32(32))).astype(np.float32)
    ang = np.stack([row[:, None] * inv, col[:, None] * inv], 1).astype(np.float32)
    cos, sin = np.cos(ang).astype(np.float32), np.sin(ang).astype(np.float32)
    C = np.stack([cos, cos], 2).reshape(n_lat, 64)
    S = np.stack([-sin, sin], 2).reshape(n_lat, 64)
    out = np.zeros((n_ctx + n_lat, 2, 64), np.float32)
    out[:n_ctx, 0, :] = 1.0
    out[n_ctx:, 0, :] = C
    out[n_ctx:, 1, :] = S
    return out


def mixer_inputs(inp, l, b, g, rope, consts):
    f = lambda a: np.ascontiguousarray(np.asarray(a, np.float32))
    win = np.asarray(inp["w_in"][l]); R0 = 768
    hs = slice(256 * g, 256 * g + 256)
    sel = np.r_[256 * g:256 * g + 256, 512 + 64 * g:512 + 64 * g + 64, 640 + 64 * g:640 + 64 * g + 64,
                R0 + 1536:R0 + 1664,
                R0 + 256 * g:R0 + 256 * g + 256, R0 + 512 + 256 * g:R0 + 512 + 256 * g + 256,
                R0 + 1024 + 256 * g:R0 + 1024 + 256 * g + 256, R0 + 1664:R0 + 1792, R0 + 1792:R0 + 1920]
    tsel = sel[384:] - R0
    taps = np.asarray(inp["shift_taps"][l])[:, tsel]
    dup = np.stack([np.concatenate([np.asarray(inp["decay_up"][l][d])[:, hs], np.asarray(inp["decay_base"][l][d])[None, hs]], 0) for d in range(2)], 1)
    iup = np.stack([np.concatenate([np.asarray(inp["iclr_up"][l][d])[:, hs], np.asarray(inp["iclr_base"][l][d])[None, hs]], 0) for d in range(2)], 1)
    qg, kg = np.asarray(inp["q_gain"][l]), np.asarray(inp["k_gain"][l])
    b256 = np.stack([np.asarray(inp["k_k"][l])[hs], np.asarray(inp["k_a"][l])[hs], np.asarray(inp["r_k"][l]).reshape(-1)[hs],
                     np.asarray(inp["ln_x_w"][l])[hs], np.asarray(inp["ln_x_b"][l])[hs]], 0)
    return {
        "xc": f(np.concatenate([np.asarray(inp["ctx"][b]), np.asarray(inp["x"][b])], 0)),
        "cvT": f(np.stack([np.asarray(inp["c"][b]), np.asarray(inp["c_ctx"])], 1)),
        "modw": f(np.asarray(inp["mod_w"][l])[:, 0:2048]),
        "modb": f(np.asarray(inp["mod_b"][l])[0:2048].reshape(16, 128).T),
        "gain": f(np.asarray(inp["norm_mix"][l]).reshape(8, 128).T),
        "win": f(win[:, sel]),
        "bc64": _bc(np.stack([qg, qg, qg, qg, kg], 0)),
        "taps": _bc(taps),
        "bc256": _bc(b256),
        "dup": f(dup), "iup": f(iup),
        "gup": f(np.asarray(inp["gate_up"][l])[:, hs]),
        "rope": rope, "consts": consts,
    }


NEG = -1.0e30
GSZ = 3


def emit_ffn(k, pb, nc, A, NTL, nctx, NCH=128):
    NTOK = NTL * 128
    xr_t, Txr = A["xr_t"], A["Txr"]
    mta, mcol, TMTa, selr = A["mta"], A["mcol"], A["TMTa"], A["selr"]
    cvT, modw, modbc, modbr, gain = A["cvT"], A["modw_f"], A["modbc"], A["modbr"], A["gain_f"]
    woutd, wqd, kTd, UTd, Vd, constd = A["wout"], A["wq"], A["kT"], A["UT"], A["Vx"], A["consts"]
    xo_t, Txo = A["xo_t"], A["Txo"]

    es = ExitStack()
    with es:
        es_outer = k.es
        k.es = es
        cst = k.sb("cst", [128, 512]); Tc = Tok()
        k.dma("sp", cst[:], constd[:, 0:512], writes=[Tc])
        ident = cst[:, 0:128]
        identb = k.sb("identb", [128, 128], BF16); Tib = Tok()
        k.op("dve", _cp(identb[:], ident), reads=[Tc], writes=[Tib])
        es_main = k.es
        sel = k.sb("sel", [128, 2]); Tsel = Tok()
        k.dma("sp", sel[:], selr[:, :], writes=[Tsel])
        modc = k.sb("modc", [128, 16, 2]); Tmodc = Tok()
        gbc = k.sb("gbc", [128, 2, 2, 1024]); Tgbc = Tok()
        woutb = k.sb("woutb", [128, 8, D], BF16); wqb = k.sb("wqb", [128, 8, 2048], BF16); TW = Tok()
        kTb = k.sb("kTb", [128, 2, 128], BF16)
        es2 = ExitStack(); k.es = es2
        cv = k.sb("cv", [128, 8, 2]); Tcv = Tok()
        k.dma("sp", cv[:], cvT.rearrange("(kc p) two -> p kc two", p=128), writes=[Tcv])
        scv = k.sb("scv", [128, 8, 2]); Tscv = Tok()
        k.op("act", _act(scv[:], cv[:], AF.Silu), reads=[Tcv], writes=[Tscv])
        mwt = k.sb("mwt", [128, 8, 512]); Tmw = Tok()
        mbc = k.sb("mbc", [128, 16]); Tmb = Tok()
        k.dma("sp", mbc[:], modbc[:, :], writes=[Tmb])
        mbr = k.sb("mbr", [1, 2048]); Tmr = Tok()
        k.dma("sp", mbr[:], modbr[:, :], writes=[Tmr])
        gt = k.sb("gt", [128, 8]); Tg = Tok()
        k.dma("sp", gt[:], gain[:, :], writes=[Tg])
        grow = k.sb("grow", [1, 512]); Tgr = Tok()
        for cb in range(8):
            k.dma("sp", mwt[:], modw[:, cb * 512:(cb + 1) * 512].rearrange("(kc p) n -> p kc n", p=128), writes=[Tmw])
            if cb in (2, 3, 4, 5):
                bank, Tb = pb.get()
                for j in range(4):
                    for kc in range(8):
                        k.op("pe", _mm(bank[:, j * 2:j * 2 + 2], mwt[:, kc, j * 128:(j + 1) * 128], scv[:, kc, :], kc == 0, kc == 7),
                             reads=[Tmw, Tscv], writes=[Tb])
                c0 = (cb - 2) * 4
                k.op("dve", _tt(modc[:, c0:c0 + 4, :], bank[:, 0:8].rearrange("p (a b) -> p a b", a=4),
                                mbc[:, c0:c0 + 4].unsqueeze(2).broadcast_to([128, 4, 2]), ALU.add), reads=[Tb, Tmb], writes=[Tmodc])
            else:
                gi = 0 if cb < 2 else 1
                half = cb % 2
                for which in range(2):
                    bank, Tb = pb.get()
                    for kc in range(8):
                        k.op("pe", _mm(bank[0:1, :], scv[:, kc, which:which + 1], mwt[:, kc, :], kc == 0, kc == 7), reads=[Tmw, Tscv], writes=[Tb])
                    k.op("dve", _tt(grow[0:1, :], bank[0:1, :], mbr[0:1, gi * 1024 + half * 512:gi * 1024 + (half + 1) * 512], ALU.add),
                         reads=[Tb, Tmr], writes=[Tgr])
                    bank2, Tb2 = pb.get()
                    k.op("pe", _mm(bank2[:, :], cst[0:1, C_ONES:C_ONES + 128], grow[0:1, :]), reads=[Tc, Tgr], writes=[Tb2])
                    k.op("act", _cp(gbc[:, which, gi, half * 512:(half + 1) * 512], bank2[:, :]), reads=[Tb2], writes=[Tgbc])
        k.op("dve", _ts(modc[:, 8:16, :], modc[:, 8:16, :], 1.0, ALU.add), reads=[Tmodc], writes=[Tmodc])
        k.op("dve", _tt(modc[:, 8:16, :], modc[:, 8:16, :], gt[:, :].unsqueeze(2).broadcast_to([128, 8, 2]), ALU.mult), reads=[Tmodc, Tg], writes=[Tmodc])
        wst = k.sb("wst", [128, 2048]); Tws = Tok()
        for kc in range(8):
            k.dma("sp", wst[:, 0:1024], woutd[kc * 128:(kc + 1) * 128, :], writes=[Tws])
            k.op("act", _cp(woutb[:, kc, :], wst[:, 0:1024]), reads=[Tws], writes=[TW])
            k.dma("sp", wst[:], wqd[kc * 128:(kc + 1) * 128, :], writes=[Tws])
            k.op("dve", _cp(wqb[:, kc, :], wst[:]), reads=[Tws], writes=[TW])
        k.dma("sp", wst[:, 0:256], kTd.rearrange("p a n -> p (a n)"), writes=[Tws])
        k.op("dve", _cp(kTb[:].rearrange("p a n -> p (a n)"), wst[:, 0:256]), reads=[Tws], writes=[TW])
        k.barrier()
        es2.close()
        k.es = es_main

        xt = k.sb("xt", [128, D]); Tx = Tok()
        mt = k.sb("mt", [128, 8, 128]); Tmt = Tok()
        mtb = k.sb("mtb", [128, 8, 128], BF16); Tmtb = Tok()
        x1 = k.sb("x1", [128, D]); Tx1 = Tok()
        sq = k.sb("sq", [128, D]); Tsq = Tok()
        ss = k.sb("ss", [128, 1]); Tss = Tok()
        hfT = k.sb("hfT", [128, 8, GSZ * 128], BF16); ThT = [Tok() for _ in range(GSZ)]
        qT = k.sb("qT", [128, 16, 128], BF16); TqT = Tok()
        S = k.sb("S", [128, 16, 128]); TS = Tok()
        tmpS = k.sb("tmpS", [128, 128]); TtS = Tok()
        v16 = k.sb("v16", [128, 16, 16]); Tv = Tok()
        cand = k.sb("cand", [128, 8, 256]); Tcand = Tok()
        c16 = k.sb("c16", [128, 8, 16]); Tc16 = Tok()
        ec = k.sb("ec", [128, 8, 16]); Tec = Tok()
        zz = k.sb("zz", [128, 8]); Tzz = Tok()
        thr = k.sb("thr", [128, 8]); Tthr = Tok()
        S2 = [k.sb("S2_%d" % i, [128, 8, 128]) for i in range(GSZ)]
        E2t = k.sb("E2t", [128, 8, 128]); TE2t = Tok()
        E1 = [k.sb("E1_%d" % i, [128, 8, 128]) for i in range(GSZ)]
        TAU = [k.sb("TAU_%d" % i, [128, 8, 128]) for i in range(GSZ)]
        TG = [Tok() for _ in range(GSZ)]
        Uf = [k.sb("Uf%d" % i, [128, 8, 128]) for i in range(2)]; TUf = [Tok() for _ in range(2)]
        Ub = [k.sb("Ub%d" % i, [128, 8, 128], BF16) for i in range(3)]; TUb = [Tok() for _ in range(3)]
        Vf = [k.sb("Vf%d" % i, [128, D]) for i in range(2)]; TVf = [Tok() for _ in range(2)]
        Vb = [k.sb("Vb%d" % i, [128, D], BF16) for i in range(3)]; TVb = [Tok() for _ in range(3)]
        Af = [k.sb("Af%d" % i, [128, 8, 128], BF16) for i in range(2)]; TAf = [Tok() for _ in range(2)]
        Ab = [k.sb("Ab%d" % i, [128, 8, 128], BF16) for i in range(2)]; TAb = [Tok() for _ in range(2)]
        E2b = [k.sb("E2b_%d" % i, [128, 8, 128], BF16) for i in range(GSZ)]
        actTb = [k.sb("actTb%d" % i, [128, GSZ * 128], BF16) for i in range(3)]; TaT = [Tok() for _ in range(3)]
        Gsb = [k.sb("Gsb%d" % i, [128, GSZ * 128], BF16) for i in range(2)]; TGsb = [Tok(), Tok()]
        aTb = [k.sb("aTb%d" % i, [128, GSZ * 128], BF16) for i in range(2)]; TaTb = [Tok(), Tok()]
        mt2 = sq[:].rearrange("p (a t) -> p a t", a=8); Tmt2 = Tsq
        banks = pb.banks; Tbk = pb.toks

        def rr(n):
            rr.i += 1
            j = 6 + rr.i % 2
            return banks[j], Tbk[j]
        rr.i = 0

        def phase1(ti, slot):
            which = 1 if ti < nctx else 0
            k.dma("sp", xt[:], xr_t(ti), reads=(Txr if isinstance(Txr, list) else [Txr]), writes=[Tx])
            c0_, c1_ = mcol(0, ti), mcol(1, ti)
            k.dma("sp", mt[:], mta(c0_).rearrange("(kc p) t -> p kc t", p=128), reads=[TMTa], writes=[Tmt])
            k.dma("sp", mt2[:], mta(c1_).rearrange("(kc p) t -> p kc t", p=128), reads=[TMTa], writes=[Tmt2])
            k.op("dve", _ts(mt[:], mt[:], sel[:, 0:1], ALU.mult), reads=[Tmt, Tsel], writes=[Tmt])
            k.op("dve", _stt(mtb[:], mt2[:], sel[:, 1:2], mt[:], ALU.mult, ALU.add), reads=[Tmt, Tmt2, Tsel], writes=[Tmtb])
            for half in range(2):
                bank, Tb = rr(0)
                for kc in range(8):
                    k.op("pe", _mm(bank[:, :], mtb[:, kc, :], woutb[:, kc, half * 512:(half + 1) * 512], kc == 0, kc == 7), reads=[Tmtb, TW], writes=[Tb])
                hs = slice(half * 512, (half + 1) * 512)
                k.op("dve", _tt(x1[:, hs], bank[:, :], gbc[:, which, 0, hs], ALU.mult), reads=[Tb, Tgbc], writes=[Tx1])
            k.op("dve", _tt(x1[:], x1[:], xt[:], ALU.add), reads=[Tx1, Tx], writes=[Tx1])
            k.dma("sp", xo_t(ti), x1[:], reads=[Tx1], writes=[Txo[ti]])
            k.op("act", _act(sq[:], x1[:], AF.Square), reads=[Tx1], writes=[Tsq])
            k.op("dve", _rs(ss[:], sq[:]), reads=[Tsq], writes=[Tss])
            k.op("dve", _ts(ss[:], ss[:], 1.0 / D, ALU.mult, 1e-6, ALU.add), reads=[Tss], writes=[Tss])
            rsqrt(k, ss[:], Tss)
            k.op("act", _act(sq[:], x1[:], AF.Copy, scale=ss[:, 0:1]), reads=[Tx1, Tss, Tsq], writes=[Tsq])
            for half in range(2):
                bank, Tb = rr(0)
                for j in range(4):
                    c0 = (half * 4 + j) * 128
                    k.op("pe", _tr(bank[:, j * 128:(j + 1) * 128], sq[:, c0:c0 + 128], ident), reads=[Tsq, Tc], writes=[Tb])
                for j in range(4):
                    kc = half * 4 + j
                    k.op("act", _act(hfT[:, kc, slot * 128:(slot + 1) * 128], bank[:, j * 128:(j + 1) * 128], AF.Identity,
                                     scale=modc[:, 8 + kc, which:which + 1], bias=modc[:, kc, which:which + 1]),
                         reads=[Tb, Tmodc], writes=[ThT[slot]])
            for qb_ in range(4):
                bank, Tb = rr(0)
                for j in range(4):
                    blk = qb_ * 4 + j
                    for kc in range(8):
                        k.op("pe", _mm(bank[:, j * 128:(j + 1) * 128], wqb[:, kc, blk * 128:(blk + 1) * 128], hfT[:, kc, slot * 128:(slot + 1) * 128], kc == 0, kc == 7),
                             reads=[TW, ThT[slot]], writes=[Tb])
                k.op("dve", _cp(qT[:, qb_ * 4:(qb_ + 1) * 4, :], bank[:, :].rearrange("p (a t) -> p a t", a=4)), reads=[Tb], writes=[TqT])
            for qb_ in range(4):
                bank, Tb = rr(0)
                for j in range(4):
                    blk = qb_ * 4 + j
                    k.op("pe", _mm(bank[:, j * 128:(j + 1) * 128], qT[:, blk, :], kTb[:, blk % 2, :]), reads=[TqT, TW], writes=[Tb])
                k.op("act", _cp(S[:, qb_ * 4:(qb_ + 1) * 4, :], bank[:, :].rearrange("p (a t) -> p a t", a=4)), reads=[Tb], writes=[TS])
            for blk in range(16):
                k.op("dve", lambda e, blk=blk: e.max(out=v16[:, blk, 0:8], in_=S[:, blk, :]), reads=[TS], writes=[Tv])
                k.op("dve", lambda e, blk=blk: e.match_replace(out=tmpS[:], in_to_replace=v16[:, blk, 0:8], in_values=S[:, blk, :], imm_value=NEG),
                     reads=[TS, Tv], writes=[TtS])
                k.op("dve", lambda e, blk=blk: e.max(out=v16[:, blk, 8:16], in_=tmpS[:]), reads=[TtS], writes=[Tv])
            vv = v16[:].rearrange("p (h a) n -> p h a n", a=2)
            k.op("dve", _tt(cand[:].rearrange("p h (a b) -> p h a b", a=16), vv[:, :, 0, :].unsqueeze(3).broadcast_to([128, 8, 16, 16]),
                            vv[:, :, 1, :].unsqueeze(2).broadcast_to([128, 8, 16, 16]), ALU.add), reads=[Tv], writes=[Tcand])
            for h in range(8):
                k.op("dve", lambda e, h=h: e.max(out=c16[:, h, 0:8], in_=cand[:, h, :]), reads=[Tcand], writes=[Tc16])
                k.op("dve", lambda e, h=h: e.match_replace(out=cand[:, h, :], in_to_replace=c16[:, h, 0:8], in_values=cand[:, h, :], imm_value=NEG),
                     reads=[Tcand, Tc16], writes=[Tcand])
                k.op("dve", lambda e, h=h: e.max(out=c16[:, h, 8:16], in_=cand[:, h, :]), reads=[Tcand], writes=[Tc16])
            k.op("dve", _tt(ec[:], c16[:], c16[:, :, 0:1].broadcast_to([128, 8, 16]), ALU.subtract), reads=[Tc16], writes=[Tec])
            k.op("act", _act(ec[:], ec[:], AF.Exp), reads=[Tec], writes=[Tec])
            k.op("dve", _rs(zz[:], ec[:]), reads=[Tec], writes=[Tzz])
            k.op("dve", lambda e: e.reciprocal(zz[:], zz[:]), reads=[Tzz], writes=[Tzz])
            k.op("dve", _ts(thr[:], c16[:, :, 15], -1e-5, ALU.add), reads=[Tc16], writes=[Tthr])
            Sv = S[:].rearrange("p (h a) n -> p h a n", a=2)
            T_ = TG[slot]
            k.op("dve", _cp(S2[slot][:], Sv[:, :, 1, :]), reads=[TS], writes=[T_])
            k.op("dve", _tt(E2t[:], Sv[:, :, 1, :], vv[:, :, 1, 0:1].broadcast_to([128, 8, 128]), ALU.subtract), reads=[TS, Tv], writes=[TE2t])
            k.op("act", _act(E2b[slot][:], E2t[:], AF.Exp), reads=[TE2t], writes=[T_])
            k.op("dve", _tt(E1[slot][:], Sv[:, :, 0, :], vv[:, :, 0, 0:1].broadcast_to([128, 8, 128]), ALU.subtract), reads=[TS, Tv], writes=[T_])
            k.op("act", _act(E1[slot][:], E1[slot][:], AF.Exp), reads=[T_], writes=[T_])
            k.op("dve", _tt(E1[slot][:], E1[slot][:], zz[:, :].unsqueeze(2).broadcast_to([128, 8, 128]), ALU.mult), reads=[T_, Tzz], writes=[T_])
            k.op("dve", _tt(TAU[slot][:], thr[:, :].unsqueeze(2).broadcast_to([128, 8, 128]), Sv[:, :, 0, :], ALU.subtract), reads=[TS, Tthr], writes=[T_])

        ngroups = (NTL + GSZ - 1) // GSZ
        cnt = 0
        for gi in range(ngroups):
            tiles = list(range(gi * GSZ, min(NTL, (gi + 1) * GSZ)))
            ng = len(tiles)
            for slot, ti in enumerate(tiles):
                phase1(ti, slot)
            def Lstage(c):
                b3 = c % 3
                f2 = c % 2
                k.dma("sp", Uf[f2][:], UTd[c], writes=[TUf[f2]])
                k.dma("sp", Vf[f2][:], Vd[c * 128:(c + 1) * 128, :], writes=[TVf[f2]])
                k.op("act", _cp(Ub[b3][:], Uf[f2][:]), reads=[TUf[f2]], writes=[TUb[b3]])
                k.op("act", _cp(Vb[b3][:], Vf[f2][:]), reads=[TVf[f2]], writes=[TVb[b3]])
                for kc in range(8):
                    k.op("pe", _mm(banks[6][:, 0:ng * 128], Ub[b3][:, kc, :], hfT[:, kc, 0:ng * 128], kc == 0, kc == 7), reads=[TUb[b3]] + ThT[:ng], writes=[Tbk[6]])
                k.op("act", _act(actTb[b3][:, 0:ng * 128], banks[6][:, 0:ng * 128], AF.Gelu), reads=[Tbk[6]], writes=[TaT[b3]])

            def Gstage(c):
                nonlocal cnt
                for slot in range(ng):
                    a4 = cnt % 2; cnt += 1
                    k.op("dve", _tt(Af[a4][:], S2[slot][:], TAU[slot][:, :, c:c + 1].broadcast_to([128, 8, 128]), ALU.is_ge), reads=[TG[slot]], writes=[TAf[a4]])
                    k.op("dve", _tt(Af[a4][:], Af[a4][:], E2b[slot][:], ALU.mult), reads=[TG[slot], TAf[a4]], writes=[TAf[a4]])
                    if slot == 2:
                        for h in range(8):
                            k.op("act", _act(Ab[a4][:, h, :], Af[a4][:, h, :], AF.Copy, scale=E1[slot][:, h, c:c + 1]), reads=[TG[slot], TAf[a4]], writes=[TAb[a4]])
                    else:
                        k.op("pool", _tt(Ab[a4][:], Af[a4][:], E1[slot][:, :, c:c + 1].broadcast_to([128, 8, 128]), ALU.mult), reads=[TG[slot], TAf[a4]], writes=[TAb[a4]])
                    for h in range(8):
                        k.op("pe", _mm(banks[7][:, slot * 128:(slot + 1) * 128], Ab[a4][:, h, :], identb[:], h == 0, h == 7), reads=[TAb[a4], Tib], writes=[Tbk[7]])
                k.op("act", _cp(Gsb[c % 2][:, 0:ng * 128], banks[7][:, 0:ng * 128]), reads=[Tbk[7]], writes=[TGsb[c % 2]])

            def Fstage(c):
                b2 = c % 2; b3 = c % 3
                k.op("dve", _tt(aTb[b2][:, 0:ng * 128], Gsb[b2][:, 0:ng * 128], actTb[b3][:, 0:ng * 128], ALU.mult), reads=[TGsb[b2], TaT[b3]], writes=[TaTb[b2]])
                for slot in range(ng):
                    for half in range(2):
                        j = slot * 2 + half
                        k.op("pe", _mm(banks[j][:, :], aTb[b2][:, slot * 128:(slot + 1) * 128], Vb[b3][:, half * 512:(half + 1) * 512], c == 0, c == NCH - 1),
                             reads=[TaTb[b2], TVb[b3]], writes=[Tbk[j]])

            Lstage(0)
            for c in range(NCH):
                if c + 1 < NCH:
                    Lstage(c + 1)
                Gstage(c)
                if c >= 1:
                    Fstage(c - 1)
            Fstage(NCH - 1)
            for slot, ti in enumerate(tiles):
                which = 1 if ti < nctx else 0
                k.dma("sp", xt[:], xo_t(ti), reads=[Txo[ti]], writes=[Tx])
                for half in range(2):
                    hs = slice(half * 512, (half + 1) * 512)
                    k.op("dve", _tt(x1[:, hs], banks[slot * 2 + half][:, :], gbc[:, which, 1, hs], ALU.mult), reads=[Tbk[slot * 2 + half], Tgbc], writes=[Tx1])
                k.op("dve", _tt(x1[:], x1[:], xt[:], ALU.add), reads=[Tx1, Tx], writes=[Tx1])
                k.dma("sp", xo_t(ti), x1[:], reads=[Tx1], writes=[Txo[ti]])
        k.barrier()
        k.es = es_outer
        print("ffn ninst", k.ninst)


def ffn_inputs(inp, l, xr, mixT, cvT, UT, consts):
    f = lambda a: np.ascontiguousarray(np.asarray(a, np.float32))
    mb = np.asarray(inp["mod_b"][l])
    return {
        "xr": f(xr), "mixT": f(mixT), "cvT": f(cvT),
        "modw": f(np.asarray(inp["mod_w"][l])[:, 2048:6144]),
        "modbc": f(mb[3072:5120].reshape(16, 128).T),
        "modbr": f(np.concatenate([mb[2048:3072], mb[5120:6144]])[None, :]),
        "gain": f(np.asarray(inp["norm_ffn"][l]).reshape(8, 128).T),
        "wout": f(inp["w_out"][l]), "wq": f(inp["peer_query"][l]),
        "kT": f(np.stack([np.asarray(inp["peer_subkeys1"][l]).T, np.asarray(inp["peer_subkeys2"][l]).T], 1)),
        "UT": UT, "Vx": f(inp["expert_v"][l]), "consts": consts,
    }


MIX_W = [("modw_m", [D, 2048]), ("modb_m", [128, 16]), ("gain_m", [128, 8]), ("win", [D, 1536]), ("bc64", [128, 5, 64]),
         ("taps", [128, 3, 1152]), ("bc256", [128, 5, 256]), ("dup", [65, 2, 256]), ("iup", [65, 2, 256]), ("gup", [128, 256])]
FFN_W = [("modw_f", [D, 4096]), ("modbc", [128, 16]), ("modbr", [1, 2048]), ("gain_f", [128, 8]), ("wout", [D, D]),
         ("wq", [D, 2048]), ("kT", [128, 2, 128]), ("UT", [128, 128, 8, 128]), ("Vx", [16384, D])]


def build_fused(NCT, NLT, depth, groups):
    NT = NCT + NLT
    TT = NT * 128
    HC, HL = NCT * 64, NLT * 64
    NTOK = HC + HL
    NTR = NTOK // 128
    CW = 3 if NT % 3 == 0 else (2 if NT % 2 == 0 else 1)
    NMC = NT // CW
    XW = 2
    NXC = (NTR + XW - 1) // XW
    nc = bass.Bass("TRN2", target_bir_lowering=False)

    def din(name, shape):
        return nc.dram_tensor(name, list(shape), F32, kind="ExternalInput").ap()

    def dscr(name, shape):
        return nc.dram_tensor(name, list(shape), F32).ap()

    G = {"xc0": din("xc0", [TT, D]), "xr0": din("xr0", [NTOK, D]), "selr": din("selr", [128, 2]),
         "cvT": din("cvT", [D, 2]), "rope": din("rope", [TT, 2, 64]), "consts": din("consts", [128, NCONST])}
    W = []
    for l in range(depth):
        W.append({n: din("%s_%d" % (n, l), shp) for n, shp in MIX_W + FFN_W})
    out = nc.dram_tensor("out", [HL, D], F32, kind="ExternalOutput").ap()
    S = {"P": dscr("Pscr", [TT + 4, 1152]), "BVG": dscr("BVG", [TT, 512]), "GH": dscr("GH", [NT, 64, 1024]),
         "RB": dscr("RB", [NT, 64, 1024]), "YB": dscr("YB", [NT, 128, 512]), "YF": dscr("YF", [NT, 128, 256]),
         "YW": dscr("YW", [NT, 128, 256])}
    MToC = [dscr("MTo%d" % c, [512, CW * 128]) for c in range(NMC)]
    MTaC = [dscr("MTa%d" % c, [1024, CW * 128]) for c in range(NMC)]
    xrows = [min(XW, NTR - c * XW) * 128 for c in range(NXC)]
    XOC = [dscr("XO%d" % c, [xrows[c], D]) for c in range(NXC)]
    XGC = [dscr("XG%d" % c, [2 * xrows[c], D]) for c in range(NXC)]
    TMTo, TMTa, TXG = Tok(), Tok(), Tok()

    def mto(i):
        return MToC[i // CW][:, (i % CW) * 128:(i % CW + 1) * 128]

    def mta(col):
        i = col // 128
        return MTaC[i // CW][:, (i % CW) * 128:(i % CW + 1) * 128]

    def xo_int(t):
        return XOC[t // XW][(t % XW) * 128:(t % XW + 1) * 128, :]

    def xg(rk, t):
        c = t // XW
        r0 = rk * xrows[c] + (t % XW) * 128
        return XGC[c][r0:r0 + 128, :]

    es = ExitStack()
    with es:
        k = K(nc, es)
        pb = PB(k)
        Txo_prev = None
        for l in range(depth):
            last = l == depth - 1
            need_ctx = not last
            A = dict(G); A.update(W[l]); A.update(S)
            A["TMTo"] = TMTo
            A["mto"], A["mta"] = mto, mta
            if l == 0:
                A["xin"], A["Txc"] = (lambda i: G["xc0"][i * 128:(i + 1) * 128, :]), Tok()
            else:
                def xin(i):
                    if i < NCT:
                        return xg(i // (NCT // 2), i % (NCT // 2))
                    j = i - NCT
                    return xg(j // (NLT // 2), HC // 128 + j % (NLT // 2))
                A["xin"], A["Txc"] = xin, TXG
            emit_mixer(k, pb, nc, A, NCT, NLT, need_ctx)
            for c in range(NMC):
                k.collective("AllGather", MToC[c], MTaC[c], groups, reads=[TMTo], writes=[TMTa])
            nctx = HC // 128 if need_ctx else 0
            NTL = nctx + HL // 128
            if l == 0:
                A["xr_t"], A["Txr"] = (lambda ti: G["xr0"][ti * 128:(ti + 1) * 128, :]), Tok()
            else:
                off = 0 if need_ctx else HC // 128
                A["xr_t"], A["Txr"] = (lambda ti, off=off: xo_int(ti + off)), Txo_prev
            A["TMTa"] = TMTa

            def mcol(h, ti, nctx=nctx):
                if ti < nctx:
                    return h * HC + ti * 128
                return NCT * 128 + h * HL + (ti - nctx) * 128
            A["mcol"] = mcol
            Txo = [Tok() for _ in range(NTL)]
            A["Txo"] = Txo
            if last:
                A["xo_t"] = lambda ti: out[ti * 128:(ti + 1) * 128, :]
            else:
                if l > 0:
                    raise NotImplementedError("depth>2 needs ping-pong XO")
                A["xo_t"] = xo_int
            emit_ffn(k, pb, nc, A, NTL, nctx)
            if not last:
                for c in range(NXC):
                    k.collective("AllGather", XOC[c], XGC[c], groups, reads=Txo, writes=[TXG])
                Txo_prev = Txo
            else:
                k.finish(Txo)
        print("fused ninst", k.ninst)
    return nc


def fused_inputs(inp, b, r, NCT, NLT, rope, consts, UTs):
    f = lambda a: np.ascontiguousarray(np.asarray(a, np.float32))
    HC, HL = NCT * 64, NLT * 64
    depth = inp["w_in"].shape[0]
    m = {"xc0": f(np.concatenate([inp["ctx"][b], inp["x"][b]], 0)),
         "xr0": f(np.concatenate([inp["ctx"][b][r * HC:(r + 1) * HC], inp["x"][b][r * HL:(r + 1) * HL]], 0)),
         "selr": _bc(np.eye(2, dtype=np.float32)[r]),
         "cvT": f(np.stack([inp["c"][b], inp["c_ctx"]], 1)), "rope": rope, "consts": consts}
    perm = np.r_[0:256, 512:768, 256:512, 768:1024]
    for l in range(depth):
        mi = mixer_inputs(inp, l, b, r, rope, consts)
        for n_, src in (("modw_m", "modw"), ("modb_m", "modb"), ("gain_m", "gain"), ("win", "win"), ("bc64", "bc64"), ("taps", "taps"),
                        ("bc256", "bc256"), ("dup", "dup"), ("iup", "iup"), ("gup", "gup")):
            m["%s_%d" % (n_, l)] = mi[src]
        mb = np.asarray(inp["mod_b"][l])
        m["modw_f_%d" % l] = f(np.asarray(inp["mod_w"][l])[:, 2048:6144])
        m["modbc_%d" % l] = f(mb[3072:5120].reshape(16, 128).T)
        m["modbr_%d" % l] = f(np.concatenate([mb[2048:3072], mb[5120:6144]])[None, :])
        m["gain_f_%d" % l] = f(np.asarray(inp["norm_ffn"][l]).reshape(8, 128).T)
        m["wout_%d" % l] = f(np.asarray(inp["w_out"][l])[perm, :])
        m["wq_%d" % l] = f(inp["peer_query"][l])
        m["kT_%d" % l] = f(np.stack([np.asarray(inp["peer_subkeys1"][l]).T, np.asarray(inp["peer_subkeys2"][l]).T], 1))
        m["UT_%d" % l] = UTs[l]
        m["Vx_%d" % l] = f(inp["expert_v"][l])
    return m


def kernel(**inp):
    inp = {kk_: np.asarray(v) for kk_, v in inp.items()}
    B, SEQ_, _ = inp["x"].shape
    CTXL = inp["ctx"].shape[1]
    depth = inp["w_in"].shape[0]
    NCT, NLT = CTXL // 128, SEQ_ // 128
    consts = make_consts()
    rope = rope_tables(CTXL, SEQ_)
    n = 2 * B
    groups = [[2 * i, 2 * i + 1] for i in range(B)]
    nc = build_fused(NCT, NLT, depth, groups)
    UTs = [np.ascontiguousarray(inp["expert_u"][l].reshape(128, 128, 8, 128).transpose(0, 3, 2, 1)) for l in range(depth)]
    maps = [fused_inputs(inp, c // 2, c % 2, NCT, NLT, rope, consts, UTs) for c in range(n)]
    res = run_bass_kernel_spmd(nc, maps, core_ids=list(range(n))).results
    HL = SEQ_ // 2
    out = np.empty((B, SEQ_, D), np.float32)
    for c in range(n):
        out[c // 2, (c % 2) * HL:(c % 2 + 1) * HL] = res[c]["out"]
    return out
```

```python
import numpy as np
import concourse.bass as bass
import concourse.mybir as mybir
from concourse.bass_utils import run_bass_kernel_spmd
from contextlib import ExitStack

F32 = mybir.dt.float32
BF16 = mybir.dt.bfloat16
I32 = mybir.dt.int32
AF = mybir.ActivationFunctionType
ALU = mybir.AluOpType
AX = mybir.AxisListType


class Tok:
    __slots__ = ("w", "r", "name")

    def __init__(self, name=""):
        self.w = None
        self.r = {}
        self.name = name


class SyncObj:
    def __init__(self, sem, step, name):
        self.sem = sem
        self.step = step
        self.count = 0
        self.name = name


class Eng:
    def __init__(self, k, name, eng, sem):
        self.k = k
        self.name = name
        self.eng = eng
        self.so = SyncObj(sem, 1, name)
        self.seen = {}
        self.n_dma = 0


class K:
    NDMASEM = 6

    def __init__(self, nc, es):
        self.nc = nc
        self.es = es
        self.es0 = es
        self.engs = {}
        for name, eng in (("pe", nc.tensor), ("act", nc.scalar), ("dve", nc.vector),
                          ("pool", nc.gpsimd), ("sp", nc.sync)):
            sem = es.enter_context(nc.semaphore("s_" + name))
            self.engs[name] = Eng(self, name, eng, sem)
        self.dmasems = {}
        for q in ("sp", "pool", "act"):
            lst = []
            for i in range(self.NDMASEM):
                sem = es.enter_context(nc.semaphore("d_%s%d" % (q, i)))
                lst.append(SyncObj(sem, 16, "d_%s%d" % (q, i)))
            self.dmasems[q] = lst
        self.ninst = 0

    def sb(self, name, shape, dt=F32):
        self.nname = getattr(self, "nname", 0) + 1
        name = "%s_%d" % (name, self.nname)
        t = self.es.enter_context(self.nc.sbuf_tensor(name, list(shape), dt))
        return t

    def ps(self, name, shape, dt=F32):
        t = self.es.enter_context(self.nc.psum_tensor(name, list(shape), dt))
        return t

    def _need(self, reads, writes):
        need = {}

        def add(so, v):
            if need.get(so, 0) < v:
                need[so] = v
        for t in reads:
            if t.w is not None:
                add(*t.w)
        for t in writes:
            if t.w is not None:
                add(*t.w)
            for so, v in t.r.items():
                add(so, v)
        return need

    def _waits(self, e, need):
        for so, v in need.items():
            if e.seen.get(so, 0) < v:
                e.eng.wait_ge(so.sem, v)
                e.seen[so] = v
                self.ninst += 1

    def _mark(self, so, val, reads, writes):
        for t in reads:
            if t.r.get(so, 0) < val:
                t.r[so] = val
        for t in writes:
            t.w = (so, val)
            t.r = {}

    def _emit(self, e, need, make):
        pend = [(so, v) for so, v in need.items() if e.seen.get(so, 0) < v]
        for so, v in pend[:-1]:
            e.eng.wait_ge(so.sem, v)
            e.seen[so] = v
            self.ninst += 1
        ins = make()
        if pend:
            so, v = pend[-1]
            ins.wait_op(so.sem, v, "sem-ge")
            e.seen[so] = v
        return ins

    def op(self, ename, fn, reads=(), writes=()):
        e = self.engs[ename]
        ins = self._emit(e, self._need(reads, writes), lambda: fn(e.eng))
        e.so.count += 1
        ins.then_inc(e.so.sem, 1)
        self._mark(e.so, e.so.count, reads, writes)
        self.ninst += 1
        return ins

    def dma(self, qname, out, in_, reads=(), writes=(), **kw):
        e = self.engs[qname]
        lst = self.dmasems[qname]
        so = lst[e.n_dma % len(lst)]
        e.n_dma += 1
        need = self._need(reads, writes)
        if so.count > 0:
            if need.get(so, 0) < so.count:
                need[so] = so.count
        ins = self._emit(e, need, lambda: e.eng.dma_start(out=out, in_=in_, **kw))
        so.count += 16
        ins.then_inc(so.sem, 16)
        self._mark(so, so.count, reads, writes)
        self.ninst += 1
        return ins

    def barrier(self):
        sos = [e.so for e in self.engs.values()] + [so for l in self.dmasems.values() for so in l]
        if hasattr(self, "ccso"):
            sos.append(self.ccso)
        for e in self.engs.values():
            need = {so: so.count for so in sos if so.count > 0}
            self._waits(e, need)

    def collective(self, kind, in_ap, out_ap, groups, reads=(), writes=()):
        e = self.engs["pool"]
        if not hasattr(self, "ccso"):
            sem = self.es0.enter_context(self.nc.semaphore("s_cc"))
            self.ccso = SyncObj(sem, 1, "cc")
        so = self.ccso
        need = self._need(reads, writes)
        if so.count > 0:
            need[so] = max(need.get(so, 0), so.count)
        self._waits(e, need)
        ins = self.nc.gpsimd.collective_compute(kind, ALU.bypass, replica_groups=groups, ins=[in_ap], outs=[out_ap])
        so.count += 1
        ins.then_inc(so.sem, 1)
        self._mark(so, so.count, reads, writes)
        self.ninst += 1

    def finish(self, toks):
        e = self.engs["sp"]
        need = self._need(list(toks), [])
        self._waits(e, need)


D = 1024
EXPM05 = 0.6065306597126334


class PB:
    def __init__(self, k):
        self.banks = [k.ps("pb%d" % i, [128, 512]) for i in range(8)]
        self.toks = [Tok("pb%d" % i) for i in range(8)]
        self.i = 0

    def get(self):
        j = self.i % 8
        self.i += 1
        return self.banks[j], self.toks[j]


def _tt(out, a, b, op):
    return lambda e: e.tensor_tensor(out=out, in0=a, in1=b, op=op)


def _ts(out, a, s1, op0, s2=None, op1=None):
    if op1 is None:
        return lambda e: e.tensor_scalar(out=out, in0=a, scalar1=s1, scalar2=None, op0=op0)
    return lambda e: e.tensor_scalar(out=out, in0=a, scalar1=s1, scalar2=s2, op0=op0, op1=op1)


def _stt(out, a, sc, b, op0, op1):
    return lambda e: e.scalar_tensor_tensor(out=out, in0=a, scalar=sc, in1=b, op0=op0, op1=op1)


def _act(out, in_, func, scale=None, bias=None):
    kw = {}
    if scale is not None:
        kw["scale"] = scale
    if bias is not None:
        kw["bias"] = bias
    return lambda e: e.activation(out=out, in_=in_, func=func, **kw)


def rsqrt(k, ap, tok):
    k.op("act", _act(ap, ap, AF.Sqrt), reads=[tok], writes=[tok])
    k.op("dve", lambda e: e.reciprocal(ap, ap), reads=[tok], writes=[tok])


def _cp(out, in_):
    return lambda e: (e.tensor_copy(out, in_) if hasattr(e, "tensor_copy") else e.copy(out, in_))


def _mm(out, lhsT, rhs, start=True, stop=True):
    return lambda e: e.matmul(out, lhsT=lhsT, rhs=rhs, start=start, stop=stop)


def _tr(out, in_, ident):
    return lambda e: e.transpose(out, in_, ident)


def _rs(out, in_):
    return lambda e: e.reduce_sum(out=out, in_=in_, axis=AX.X)


def make_consts():
    I = np.eye(128, dtype=np.float32)
    Uq = np.triu(np.ones((128, 128), np.float32))
    Lq = np.tril(np.ones((128, 128), np.float32))
    U = Uq - I
    L = Lq - I
    ones = np.ones((128, 128), np.float32)
    mAB_f = np.concatenate([-U, -Uq, U, Uq], 1)
    mAB_b = np.concatenate([-L, -Lq, L, Lq], 1)
    mC_f = np.concatenate([-L] * 4, 1)
    mC_b = np.concatenate([-U] * 4, 1)
    return np.ascontiguousarray(np.concatenate([I, Uq, Lq, ones, mAB_f, mAB_b, mC_f, mC_b], 1))


C_I, C_UQ, C_LQ, C_ONES, C_MABF, C_MABB, C_MCF, C_MCB = 0, 128, 256, 384, 512, 1024, 1536, 2048
NCONST = 2560


def emit_mixer(k, pb, nc, A, NCT, NLT, need_ctx):
    NT = NCT + NLT
    TT = NT * 128
    xin, Txc = A["xin"], A["Txc"]
    cvT, modw, modb, gain, win = A["cvT"], A["modw_m"], A["modb_m"], A["gain_m"], A["win"]
    bc64, tapsd, bc256, dupd, iupd, gupd = A["bc64"], A["taps"], A["bc256"], A["dup"], A["iup"], A["gup"]
    roped, constd = A["rope"], A["consts"]
    mto = A["mto"]
    P, BVG, GH, RB, YB, YF, YW = A["P"], A["BVG"], A["GH"], A["RB"], A["YB"], A["YF"], A["YW"]
    TMTo = A["TMTo"]

    def poff(i):
        return 1 + 128 * i if i < NCT else 3 + 128 * i

    es = ExitStack()
    with es:
        es_outer = k.es
        k.es = es
        cst = k.sb("cst", [128, NCONST]); Tc = Tok("cst")
        k.dma("sp", cst[:], constd[:, :], writes=[Tc])
        ident = cst[:, C_I:C_I + 128]
        ones128 = cst[:, C_ONES:C_ONES + 128]

        es_main = k.es
        es2 = ExitStack()
        k.es = es2
        cv = k.sb("cv", [128, 8, 2]); Tcv = Tok()
        k.dma("sp", cv[:], cvT.rearrange("(kc p) two -> p kc two", p=128), writes=[Tcv])
        scv = k.sb("scv", [128, 8, 2]); Tscv = Tok()
        k.op("act", _act(scv[:], cv[:], AF.Silu), reads=[Tcv], writes=[Tscv])
        mwt = k.sb("mwt", [128, 8, 512]); Tmw = Tok()
        mod = k.sb("mod", [128, 16, 2]); Tmod = Tok()
        mbt = k.sb("mbt", [128, 16]); Tmb = Tok()
        k.dma("sp", mbt[:], modb[:, :], writes=[Tmb])
        gt = k.sb("gt", [128, 8]); Tg = Tok()
        k.dma("sp", gt[:], gain[:, :], writes=[Tg])
        for cb in range(4):
            k.dma("sp", mwt[:], modw[:, cb * 512:(cb + 1) * 512].rearrange("(kc p) n -> p kc n", p=128), writes=[Tmw])
            bank, Tb = pb.get()
            for j in range(4):
                for kc in range(8):
                    k.op("pe", _mm(bank[:, j * 2:j * 2 + 2], mwt[:, kc, j * 128:(j + 1) * 128], scv[:, kc, :], kc == 0, kc == 7),
                         reads=[Tmw, Tscv], writes=[Tb])
            k.op("dve", _tt(mod[:, cb * 4:(cb + 1) * 4, :], bank[:, 0:8].rearrange("p (a b) -> p a b", a=4),
                            mbt[:, cb * 4:(cb + 1) * 4].unsqueeze(2).broadcast_to([128, 4, 2]), ALU.add),
                 reads=[Tb, Tmb], writes=[Tmod])
        amod = k.sb("amod", [128, 8, 2]); Tam = Tok()
        k.op("dve", _ts(amod[:], mod[:, 8:16, :], 1.0, ALU.add), reads=[Tmod], writes=[Tam])
        k.op("dve", _tt(amod[:], amod[:], gt[:, :].unsqueeze(2).broadcast_to([128, 8, 2]), ALU.mult), reads=[Tam, Tg], writes=[Tam])

        Wb = k.sb("Wb", [128, 8, 1536], BF16); TWb = Tok()
        wraw = k.sb("wraw", [128, 1536]); Twr = Tok()
        biasrow = k.sb("biasrow", [1, 1536]); Tbr = Tok()
        biasbc = k.sb("biasbc", [128, 1536]); Tbb = Tok()

        def prep_weights(which):
            banks = [pb.get() for _ in range(3)]
            for kc in range(8):
                k.dma("sp", wraw[:], win[kc * 128:(kc + 1) * 128, :], writes=[Twr])
                k.op("act", _act(Wb[:, kc, :], wraw[:], AF.Copy, scale=amod[:, kc, which:which + 1]), reads=[Twr, Tam], writes=[TWb])
                for g in range(3):
                    k.op("pe", _mm(banks[g][0][0:1, :], mod[:, kc, which:which + 1], wraw[:, g * 512:(g + 1) * 512], kc == 0, kc == 7),
                         reads=[Tmod, Twr], writes=[banks[g][1]])
            for g in range(3):
                k.op("dve", _cp(biasrow[0:1, g * 512:(g + 1) * 512], banks[g][0][0:1, :]), reads=[banks[g][1]], writes=[Tbr])
            for g in range(3):
                bank, Tb = pb.get()
                k.op("pe", _mm(bank[:, :], cst[0:1, C_ONES:C_ONES + 128], biasrow[0:1, g * 512:(g + 1) * 512]), reads=[Tc, Tbr], writes=[Tb])
                k.op("dve", _cp(biasbc[:, g * 512:(g + 1) * 512], bank[:, :]), reads=[Tb], writes=[Tbb])

        QT = k.sb("QT", [64, 4, TT], BF16); TQ = [Tok() for _ in range(NT)]
        KTt = k.sb("KTt", [64, TT], BF16); TK = [Tok() for _ in range(NT)]
        Vt = k.sb("Vt", [128, NT, 65], BF16); TV = [Tok() for _ in range(NT)]
        Tv1 = Tok()
        k.op("dve", lambda e: e.memset(Vt[:, :, 64:65], 1.0), writes=[Tv1])
        g5 = k.sb("g5", [128, 5, 64]); Tg5 = Tok()
        k.dma("sp", g5[:], bc64[:, :, :], writes=[Tg5])
        X2 = [k.sb("xt%d" % i, [128, D]) for i in range(2)]; TX = [Tok(), Tok()]
        sqj = k.sb("sqj", [128, D]); Tsq = Tok()
        xn = k.sb("xn", [128, D]); Txn = Tok()
        ss = k.sb("ss", [128, 1]); Tss = Tok()
        xT = k.sb("xT", [128, 8, 128], BF16); TxT = Tok()
        pt = k.sb("pt", [128, 1536]); Tp = Tok()
        rp = k.sb("rp", [128, 2, 64]); Trp = Tok()
        ss5 = k.sb("ss5", [128, 5]); Ts5 = Tok()
        qn = k.sb("qn", [128, 5, 64]); Tqn = Tok()
        qa = k.sb("qa", [128, 5, 64]); Tqa = Tok()
        qb = k.sb("qb", [128, 5, 64]); Tqb = Tok()
        zrow = k.sb("zrow", [1, 1152]); Tzr = Tok()
        k.op("dve", lambda e: e.memset(zrow[:], 0.0), writes=[Tzr])
        TPz = Tok()
        for r_ in (0, NCT * 128 + 1, NCT * 128 + 2, TT + 3):
            k.dma("sp", P[r_:r_ + 1, :], zrow[0:1, :], reads=[Tzr], writes=[TPz])
        TP = [Tok() for _ in range(NT)]

        def phaseA(i):
            which = 1 if i < NCT else 0
            xt = X2[i % 2]; Tx = TX[i % 2]
            k.dma("sp", xt[:], xin(i), reads=[Txc], writes=[Tx])
            k.dma("sp", rp[:], roped[i * 128:(i + 1) * 128, :, :], writes=[Trp])
            k.op("act", _act(sqj[:], xt[:], AF.Square), reads=[Tx], writes=[Tsq])
            k.op("dve", _rs(ss[:], sqj[:]), reads=[Tsq], writes=[Tss])
            k.op("dve", _ts(ss[:], ss[:], 1.0 / D, ALU.mult, 1e-6, ALU.add), reads=[Tss], writes=[Tss])
            rsqrt(k, ss[:], Tss)
            k.op("act", _act(xn[:], xt[:], AF.Copy, scale=ss[:, 0:1]), reads=[Tx, Tss], writes=[Txn])
            for half in range(2):
                bank, Tb = pb.get()
                for j in range(4):
                    c0 = (half * 4 + j) * 128
                    k.op("pe", _tr(bank[:, j * 128:(j + 1) * 128], xn[:, c0:c0 + 128], ident), reads=[Txn, Tc], writes=[Tb])
                k.op("dve" if half == 0 else "act", _cp(xT[:, half * 4:(half + 1) * 4, :], bank[:, :].rearrange("p (a b) -> p a b", a=4)),
                     reads=[Tb], writes=[TxT])
            for g in range(3):
                bank, Tb = pb.get()
                for kc in range(8):
                    k.op("pe", _mm(bank[:, :], xT[:, kc, :], Wb[:, kc, g * 512:(g + 1) * 512], kc == 0, kc == 7), reads=[TxT, TWb], writes=[Tb])
                k.op("dve", _tt(pt[:, g * 512:(g + 1) * 512], bank[:, :], biasbc[:, g * 512:(g + 1) * 512], ALU.add), reads=[Tb, Tbb], writes=[Tp])
            k.dma("sp", P[poff(i):poff(i) + 128, :], pt[:, 384:1536], reads=[Tp], writes=[TP[i]])
            q5 = pt[:, 0:320].rearrange("p (h d) -> p h d", h=5)
            k.op("dve", _tt(qa[:], q5, q5, ALU.mult), reads=[Tp], writes=[Tqa])
            k.op("dve", _rs(ss5[:], qa[:]), reads=[Tqa], writes=[Ts5])
            k.op("dve", _ts(ss5[:], ss5[:], 1.0 / 64, ALU.mult, 1e-6, ALU.add), reads=[Ts5], writes=[Ts5])
            rsqrt(k, ss5[:], Ts5)
            k.op("dve", _tt(qn[:], q5, ss5[:, :].unsqueeze(2).broadcast_to([128, 5, 64]), ALU.mult), reads=[Tp, Ts5], writes=[Tqn])
            k.op("dve", _tt(qn[:], qn[:], g5[:], ALU.mult), reads=[Tqn, Tg5], writes=[Tqn])
            k.op("dve", _tt(qa[:], qn[:], rp[:, 0, :].unsqueeze(1).broadcast_to([128, 5, 64]), ALU.mult), reads=[Tqn, Trp], writes=[Tqa])
            qn6 = qn[:].rearrange("p h (a b f) -> p h a b f", a=2, b=2)
            qb6 = qb[:].rearrange("p h (a b f) -> p h a b f", a=2, b=2)
            rs6 = rp[:, 1, :].rearrange("p (a b f) -> p a b f", a=2, b=2)
            for hb in range(2):
                k.op("dve", _tt(qb6[:, :, :, hb, :], qn6[:, :, :, 1 - hb, :],
                                rs6[:, :, hb, :].unsqueeze(1).broadcast_to([128, 5, 2, 16]), ALU.mult),
                     reads=[Tqn, Trp], writes=[Tqb])
            k.op("dve", _tt(qa[:], qa[:], qb[:], ALU.add), reads=[Tqa, Tqb], writes=[Tqa])
            bank, Tb = pb.get()
            for h in range(4):
                k.op("pe", _tr(bank[0:64, h * 128:(h + 1) * 128], qa[:, h, :], ident), reads=[Tqa, Tc], writes=[Tb])
            k.op("act", _cp(QT[:, :, i * 128:(i + 1) * 128], bank[0:64, :].rearrange("p (h t) -> p h t", h=4)), reads=[Tb], writes=[TQ[i]])
            bank, Tb = pb.get()
            k.op("pe", _tr(bank[0:64, 0:128], qa[:, 4, :], ident), reads=[Tqa, Tc], writes=[Tb])
            k.op("act", _cp(KTt[:, i * 128:(i + 1) * 128], bank[0:64, 0:128]), reads=[Tb], writes=[TK[i]])
            k.op("dve", _cp(Vt[:, i, 0:64], pt[:, 320:384]), reads=[Tp, Tv1], writes=[TV[i]])

        if NCT > 0:
            prep_weights(1)
            for i in range(NCT):
                phaseA(i)
        prep_weights(0)
        for i in range(NCT, NT):
            phaseA(i)

        PTs = [k.sb("PT%d" % i, [128, 512], BF16) for i in range(2)]; TPT = [Tok(), Tok()]
        Osb = k.sb("Osb", [65, 512]); TO = Tok()
        At = k.sb("At", [64, 512]); TAt = Tok()
        Tout = TMTo
        qtiles = list(range(NCT, NT)) + (list(range(NCT)) if need_ctx else [])
        for qi in qtiles:
            keys = list(range(NT)) if qi >= NCT else list(range(NCT))
            po, Tpo = pb.get()
            def issue_S(n):
                kt_ = keys[n]
                ps, Tps = pb.get()
                if ps is po:
                    ps, Tps = pb.get()
                k.op("pe", _mm(ps[:, :], KTt[:, kt_ * 128:(kt_ + 1) * 128], QT[:, :, qi * 128:(qi + 1) * 128]), reads=[TK[kt_], TQ[qi]], writes=[Tps])
                return ps, Tps
            cur = issue_S(0)
            for n, kt in enumerate(keys):
                nxt = issue_S(n + 1) if n + 1 < len(keys) else None
                ps, Tps = cur
                k.op("act", _act(PTs[n % 2][:], ps[:, :], AF.Exp, scale=0.125), reads=[Tps], writes=[TPT[n % 2]])
                k.op("pe", _mm(po[0:65, :], Vt[:, kt, :], PTs[n % 2][:], n == 0, n == len(keys) - 1), reads=[TV[kt], TPT[n % 2]], writes=[Tpo])
                cur = nxt
            k.op("dve", _cp(Osb[0:65, :], po[0:65, :]), reads=[Tpo], writes=[TO])
            k.op("dve", lambda e: e.reciprocal(Osb[64:65, :], Osb[64:65, :]), reads=[TO], writes=[TO])
            bank, Tb = pb.get()
            k.op("pe", _mm(bank[0:64, :], cst[64:65, C_ONES:C_ONES + 64], Osb[64:65, :]), reads=[Tc, TO], writes=[Tb])
            k.op("dve", _tt(At[:, :], Osb[0:64, :], bank[0:64, :], ALU.mult), reads=[TO, Tb], writes=[TAt])
            k.dma("sp", mto(qi)[0:256, :].rearrange("(h d) t -> d h t", h=4), At[:, :].rearrange("p (h t) -> p h t", h=4), reads=[TAt], writes=[Tout])

        k.barrier()
        es2.close()
        k.es = es_main
        build_rwkv(k, pb, nc, locals())
        k.barrier()
        k.es = es_outer
        print("mixer ninst", k.ninst)


def build_rwkv(k, pb, nc, L):
    cst, Tc, ident = L["cst"], L["Tc"], L["ident"]
    P, TP, TPz, poff = L["P"], L["TP"], L["TPz"], L["poff"]
    BVG, GH, RB, YB, YF, YW = L["BVG"], L["GH"], L["RB"], L["YB"], L["YF"], L["YW"]
    mto, TMTo = L["mto"], L["TMTo"]
    NCT, NT, need_ctx = L["NCT"], L["NT"], L["need_ctx"]
    ones128 = cst[:, C_ONES:C_ONES + 128]

    taps = k.sb("tapsb", [128, 3, 1152]); Ttp = Tok()
    k.dma("sp", taps[:], L["tapsd"][:, :, :], writes=[Ttp])
    b256 = k.sb("b256", [128, 5, 256]); Tb2 = Tok()
    k.dma("sp", b256[:], L["bc256"][:, :, :], writes=[Tb2])
    kk_bc, ka_bc, rk_bc, lnw_bc, lnb_bc = (b256[:, j, :] for j in range(5))
    omka = k.sb("omka", [128, 256]); Tom = Tok()
    k.op("dve", _ts(omka[:], ka_bc, -1.0, ALU.mult, 1.0, ALU.add), reads=[Tb2], writes=[Tom])
    dupw = k.sb("dupw", [65, 2, 256]); iupw = k.sb("iupw", [65, 2, 256]); gupw = k.sb("gupw", [128, 256]); Tw = Tok()
    k.dma("sp", dupw[:], L["dupd"][:, :, :], writes=[Tw])
    k.dma("sp", iupw[:], L["iupd"][:, :, :], writes=[Tw])
    k.dma("sp", gupw[:], L["gupd"][:, :], writes=[Tw])

    Pm = k.sb("Pm", [128, 1152]); P0 = k.sb("P0", [128, 1152]); Pp = k.sb("Pp", [128, 1152]); TPl = Tok()
    cols = k.sb("cols", [128, 1152]); Tco = Tok()
    wdt = k.sb("wdt", [128, 128]); sg = k.sb("sg", [128, 128]); Twd = Tok()
    LT = k.sb("LT", [65, 4, 128]); TLT = Tok()
    TLo = Tok()
    k.op("dve", lambda e: e.memset(LT[64:65, :, :], 1.0), writes=[TLo])
    sgT = k.sb("sgT", [128, 128]); TsgT = Tok()
    lw = k.sb("lw", [128, 512]); Tlw = Tok()
    aa = k.sb("aa", [128, 512]); Taa = Tok()
    bvg = k.sb("bvg", [128, 512]); Tbvg = Tok()
    kkt = k.sb("kkt", [128, 256]); Tkk = Tok()
    t256 = k.sb("t256", [128, 256]); Tt2 = Tok()
    s4 = k.sb("s4", [128, 4]); Ts4 = Tok()
    kd = k.sb("kd", [128, 512]); Tkd = Tok()
    bb = k.sb("bb", [128, 512]); Tbb_ = Tok()
    ecp = k.sb("ecp", [128, 512]); ecm = k.sb("ecm", [128, 512]); iw = k.sb("iw", [128, 512]); etot = k.sb("etot", [128, 512])
    Te = Tok()
    dcol = k.sb("dcol", [64, 8]); Tdc = Tok()
    al = k.sb("al", [128, 512]); be = k.sb("be", [128, 512]); ka_ = k.sb("ka_", [128, 512]); rho = k.sb("rho", [128, 512])
    bepn = k.sb("bepn", [128, 512]); kap = k.sb("kap", [128, 512]); Tf = Tok()
    ART = k.sb("ART", [128, 4, 2, 128]); BTt = k.sb("BTt", [128, 4, 128]); KTk = k.sb("KTk", [128, 4, 128]); TT_ = Tok()
    XM = k.sb("XM", [128, 8, 512]); TXM = [Tok() for _ in range(8)]
    XT0 = k.sb("XT0", [128, 8, 128]); TXT0h = [Tok(), Tok()]
    XS = [k.sb("XS%d" % i, [128, 8, 128]) for i in range(2)]; TXSh = [[Tok(), Tok()], [Tok(), Tok()]]
    XTS = [k.sb("XTS%d" % i, [128, 8, 128]) for i in range(2)]; TXTSh = [[Tok(), Tok()], [Tok(), Tok()]]
    Z = k.sb("Z", [128, 8, 128]); TZh = [Tok(), Tok()]
    Rbs = k.sb("Rbs", [64, 1024]); TRbs = Tok()
    Ybs = k.sb("Ybs", [128, 512]); TYbs = Tok()
    GHs = k.sb("GHs", [64, 1024]); TGHs = Tok()
    TBVG = [Tok() for _ in range(NT)]
    TGH = [Tok() for _ in range(NT)]; TRB = [Tok() for _ in range(NT)]; TYB = [Tok() for _ in range(NT)]

    def bc_d(ap256):
        return ap256.unsqueeze(1).broadcast_to([128, 2, 256])

    def v3(ap512):
        return ap512.rearrange("p (d c) -> p d c", d=2)

    def h4(ap256):
        return ap256.rearrange("p (h c) -> p h c", h=4)

    def phaseB(i):
        o = poff(i)
        rd = [TP[i], TPz] + ([TP[i - 1]] if i > 0 else []) + ([TP[i + 1]] if i + 1 < NT else [])
        k.dma("sp", Pm[:], P[o - 1:o + 127, :], reads=rd, writes=[TPl])
        k.dma("sp", P0[:], P[o:o + 128, :], reads=rd, writes=[TPl])
        k.dma("sp", Pp[:], P[o + 1:o + 129, :], reads=rd, writes=[TPl])
        k.op("dve", _tt(cols[:], Pm[:], taps[:, 0, :], ALU.mult), reads=[TPl, Ttp], writes=[Tco])
        k.op("dve", _tt(P0[:], P0[:], taps[:, 1, :], ALU.mult), reads=[TPl, Ttp], writes=[TPl])
        k.op("dve", _tt(Pp[:], Pp[:], taps[:, 2, :], ALU.mult), reads=[TPl, Ttp], writes=[TPl])
        k.op("dve", _tt(cols[:], cols[:], P0[:], ALU.add), reads=[TPl, Tco], writes=[Tco])
        k.op("dve", _tt(cols[:], cols[:], Pp[:], ALU.add), reads=[TPl, Tco], writes=[Tco])
        wd = cols[:, 0:128]; r_ = cols[:, 128:384]; kr = cols[:, 384:640]; vr = cols[:, 640:896]
        ad = cols[:, 896:1024]; gd = cols[:, 1024:1152]
        k.op("act", _act(wdt[:], wd, AF.Tanh), reads=[Tco], writes=[Twd])
        k.op("act", _act(sg[:], gd, AF.Sigmoid), reads=[Tco], writes=[Twd])
        bank, Tb = pb.get()
        for d in range(2):
            k.op("pe", _tr(bank[0:64, d * 128:(d + 1) * 128], wdt[:, d * 64:(d + 1) * 64], ident), reads=[Twd, Tc], writes=[Tb])
            k.op("pe", _tr(bank[0:64, (2 + d) * 128:(3 + d) * 128], cols[:, 896 + d * 64:896 + (d + 1) * 64], ident), reads=[Tco, Tc], writes=[Tb])
        k.op("dve", _cp(LT[0:64, :, :], bank[0:64, :].rearrange("p (a t) -> p a t", a=4)), reads=[Tb, TLo], writes=[TLT])
        bank, Tb = pb.get()
        k.op("pe", _tr(bank[:, 0:128], sg[:], ident), reads=[Twd, Tc], writes=[Tb])
        k.op("act", _cp(sgT[:], bank[:, 0:128]), reads=[Tb], writes=[TsgT])
        bz, Tbz = pb.get()
        bi, Tbi = pb.get()
        bg, Tbg = pb.get()
        for d in range(2):
            k.op("pe", _mm(bz[:, d * 256:(d + 1) * 256], LT[0:65, d, :], dupw[0:65, d, :]), reads=[TLT, Tw], writes=[Tbz])
            k.op("pe", _mm(bi[:, d * 256:(d + 1) * 256], LT[0:65, 2 + d, :], iupw[0:65, d, :]), reads=[TLT, Tw], writes=[Tbi])
        k.op("pe", _mm(bg[:, 0:256], sgT[:], gupw[:]), reads=[TsgT, Tw], writes=[Tbg])
        k.op("act", _act(lw[:], bz[:, :], AF.Sigmoid), reads=[Tbz], writes=[Tlw])
        k.op("dve", _ts(lw[:], lw[:], -EXPM05, ALU.mult), reads=[Tlw], writes=[Tlw])
        k.op("act", _act(aa[:], bi[:, :], AF.Sigmoid), reads=[Tbi], writes=[Taa])
        k.op("act", _cp(bvg[:, 256:512], bg[:, 0:256]), reads=[Tbg], writes=[Tbvg])
        k.op("dve", _tt(kkt[:], kr, kk_bc, ALU.mult), reads=[Tco, Tb2], writes=[Tkk])
        k.op("dve", _tt(t256[:], kkt[:], kkt[:], ALU.mult), reads=[Tkk], writes=[Tt2])
        k.op("dve", _rs(s4[:], h4(t256[:])), reads=[Tt2], writes=[Ts4])
        k.op("dve", _ts(s4[:], s4[:], 1e-12, ALU.add), reads=[Ts4], writes=[Ts4])
        rsqrt(k, s4[:], Ts4)
        k.op("dve", _tt(h4(kkt[:]), h4(kkt[:]), s4[:, :].unsqueeze(2).broadcast_to([128, 4, 64]), ALU.mult), reads=[Tkk, Ts4], writes=[Tkk])
        k.op("dve", _tt(v3(kd[:]), v3(aa[:]), bc_d(ka_bc), ALU.mult), reads=[Taa, Tb2], writes=[Tkd])
        k.op("dve", _tt(v3(kd[:]), v3(kd[:]), bc_d(omka[:]), ALU.add), reads=[Tkd, Tom], writes=[Tkd])
        k.op("dve", _tt(v3(kd[:]), v3(kd[:]), bc_d(kr), ALU.mult), reads=[Tkd, Tco], writes=[Tkd])
        k.op("dve", _tt(v3(bb[:]), v3(aa[:]), bc_d(kkt[:]), ALU.mult), reads=[Taa, Tkk], writes=[Tbb_])
        k.op("dve", _tt(t256[:], r_, kr, ALU.mult), reads=[Tco, Ts4], writes=[Tt2])
        k.op("dve", _tt(t256[:], t256[:], rk_bc, ALU.mult), reads=[Tt2, Tb2], writes=[Tt2])
        k.op("dve", _rs(s4[:], h4(t256[:])), reads=[Tt2, Tkk], writes=[Ts4])
        k.op("dve", _tt(h4(bvg[:, 0:256]), h4(vr), s4[:, :].unsqueeze(2).broadcast_to([128, 4, 64]), ALU.mult), reads=[Tco, Ts4], writes=[Tbvg])
        k.dma("sp", BVG[i * 128:(i + 1) * 128, :], bvg[:], reads=[Tbvg], writes=[TBVG[i]])
        bc_, Tbc = pb.get()
        bt_, Tbt = pb.get()
        bd_, Tbd = pb.get()
        k.op("pe", _mm(bc_[:, 0:256], cst[:, C_UQ:C_UQ + 128], lw[:, 0:256]), reads=[Tc, Tlw], writes=[Tbc])
        k.op("pe", _mm(bc_[:, 256:512], cst[:, C_LQ:C_LQ + 128], lw[:, 256:512]), reads=[Tc, Tlw], writes=[Tbc])
        k.op("pe", _mm(bt_[:, :], ones128, lw[:, :]), reads=[Tc, Tlw], writes=[Tbt])
        for u in range(8):
            k.op("pe", _mm(bd_[0:64, u:u + 1], lw[:, u * 64:(u + 1) * 64], cst[:, C_ONES:C_ONES + 1]), reads=[Tc, Tlw], writes=[Tbd])
        k.op("act", _act(ecp[:], bc_[:, :], AF.Exp), reads=[Tbc], writes=[Te])
        k.op("act", _act(ecm[:], bc_[:, :], AF.Exp, scale=-1.0), reads=[Tbc], writes=[Te])
        k.op("act", _act(iw[:], lw[:], AF.Exp, scale=-1.0), reads=[Tlw], writes=[Te])
        k.op("act", _act(etot[:], bt_[:, :], AF.Exp), reads=[Tbt], writes=[Te])
        k.op("act", _act(dcol[:, :], bd_[0:64, 0:8], AF.Exp), reads=[Tbd], writes=[Tdc])
        k.op("dve", _tt(iw[:], iw[:], ecp[:], ALU.mult), reads=[Te], writes=[Te])
        k.op("dve", _tt(etot[:], etot[:], ecm[:], ALU.mult), reads=[Te], writes=[Te])
        k.op("dve", _tt(v3(al[:]), v3(iw[:]), bc_d(kkt[:]), ALU.mult), reads=[Te, Tkk], writes=[Tf])
        k.op("dve", _tt(be[:], bb[:], ecm[:], ALU.mult), reads=[Te, Tbb_], writes=[Tf])
        k.op("dve", _tt(ka_[:], kd[:], ecm[:], ALU.mult), reads=[Te, Tkd], writes=[Tf])
        k.op("dve", _tt(v3(rho[:]), v3(ecp[:]), bc_d(r_), ALU.mult), reads=[Te, Tco], writes=[Tf])
        k.op("dve", _stt(bepn[:], bb[:], -1.0, etot[:], ALU.mult, ALU.mult), reads=[Te, Tbb_], writes=[Tf])
        k.op("dve", _tt(kap[:], kd[:], etot[:], ALU.mult), reads=[Te, Tkd], writes=[Tf])
        for src, dst in ((al, lambda: ART[:, :, 0, :]), (rho, lambda: ART[:, :, 1, :]), (be, lambda: BTt[:, :, :]), (ka_, lambda: KTk[:, :, :])):
            bank, Tb = pb.get()
            for pr in range(4):
                k.op("pe", _tr(bank[:, pr * 128:(pr + 1) * 128], src[:, pr * 128:(pr + 1) * 128], ident), reads=[Tf, Tc], writes=[Tb])
            k.op("act", _cp(dst(), bank[:, :].rearrange("p (a t) -> p a t", a=4)), reads=[Tb], writes=[TT_])
        for half in range(2):
            mab = cst[:, (C_MABF if half == 0 else C_MABB):(C_MABF if half == 0 else C_MABB) + 512]
            mc = cst[:, (C_MCF if half == 0 else C_MCB):(C_MCF if half == 0 else C_MCB) + 512]
            bC, TbC = pb.get()
            for uu in range(4):
                u = half * 4 + uu
                pr = u // 2; rws = slice((u % 2) * 64, (u % 2) * 64 + 64)
                bA, TbA = pb.get()
                if bA is bC:
                    bA, TbA = pb.get()
                arr = ART[rws, pr, :, :].rearrange("p a t -> p (a t)")
                k.op("pe", _mm(bA[:, 0:256], BTt[rws, pr, :], arr), reads=[TT_], writes=[TbA])
                k.op("pe", _mm(bA[:, 256:512], KTk[rws, pr, :], arr), reads=[TT_], writes=[TbA])
                k.op("pe", _mm(bC[:, uu * 128:(uu + 1) * 128], ART[rws, pr, 0, :], BTt[rws, pr, :]), reads=[TT_], writes=[TbC])
                k.op("dve", _tt(XM[:, u, :], bA[:, :], mab, ALU.mult), reads=[TbA, Tc], writes=[TXM[u]])
            k.op("dve", _tt(XT0[:, half * 4:(half + 1) * 4, :], bC[:, :].rearrange("p (a t) -> p a t", a=4),
                            mc.rearrange("p (a t) -> p a t", a=4), ALU.mult), reads=[TbC, Tc], writes=[TXT0h[half]])
        bank, Tb = pb.get()
        for u in range(8):
            h = u % 4
            k.op("pe", _mm(bank[:, u * 64:(u + 1) * 64], XM[:, u, 256:384], cols[:, 640 + h * 64:640 + (h + 1) * 64]), reads=[TXM[u], Tco], writes=[Tb])
        k.op("dve", _cp(Z[:, :, 64:128], bank[:, :].rearrange("p (u c) -> p u c", u=8)), reads=[Tb], writes=TZh)
        k.op("act", _cp(Z[:, :, 0:64], al[:].rearrange("p (u c) -> p u c", u=8)), reads=[Tf] + TZh, writes=TZh)
        NR = 7
        for m in range(NR):
            pend = []
            for hb in range(2):
                if m == 0:
                    Xc = lambda u: XM[:, u, 0:128]
                    XTc = lambda u: XT0[:, u, :]
                    rX = lambda u: [TXM[u]]
                    rXT = lambda u, hb=hb: [TXT0h[hb]]
                else:
                    Xc = (lambda mm_: (lambda u: XS[mm_ % 2][:, u, :]))(m)
                    XTc = (lambda mm_: (lambda u: XTS[mm_ % 2][:, u, :]))(m)
                    rX = (lambda mm_, hb=hb: (lambda u: [TXSh[mm_ % 2][hb]]))(m)
                    rXT = (lambda mm_, hb=hb: (lambda u: [TXTSh[mm_ % 2][hb]]))(m)
                bk, Tbk_ = pb.get()
                for uu in range(4):
                    u = hb * 4 + uu
                    k.op("pe", _mm(bk[:, uu * 128:(uu + 1) * 128], Xc(u), Z[:, u, :]), reads=rX(u) + [TZh[hb]], writes=[Tbk_])
                bx = bxt = None
                if m < NR - 1:
                    bx = pb.get(); bxt = pb.get()
                    for uu in range(4):
                        u = hb * 4 + uu
                        sl = slice(uu * 128, (uu + 1) * 128)
                        k.op("pe", _mm(bx[0][:, sl], XTc(u), Xc(u)), reads=rX(u) + rXT(u), writes=[bx[1]])
                        k.op("pe", _mm(bxt[0][:, sl], Xc(u), XTc(u)), reads=rX(u) + rXT(u), writes=[bxt[1]])
                pend.append((hb, bk, Tbk_, bx, bxt))
            for hb, bk, Tbk_, bx, bxt in pend:
                hs_ = slice(hb * 4, (hb + 1) * 4)
                k.op("dve", _tt(Z[:, hs_, :], Z[:, hs_, :], bk[:, :].rearrange("p (a t) -> p a t", a=4), ALU.add), reads=[Tbk_, TZh[hb]], writes=[TZh[hb]])
                if m < NR - 1:
                    k.op("act", _cp(XS[(m + 1) % 2][:, hs_, :], bx[0][:, :].rearrange("p (a t) -> p a t", a=4)), reads=[bx[1]], writes=[TXSh[(m + 1) % 2][hb]])
                    k.op("dve" if hb == 0 else "act", _cp(XTS[(m + 1) % 2][:, hs_, :], bxt[0][:, :].rearrange("p (a t) -> p a t", a=4)),
                         reads=[bxt[1]], writes=[TXTSh[(m + 1) % 2][hb]])
        br = [pb.get(), pb.get()]
        by, Tby = pb.get()
        bgm, Tbgm = pb.get()
        bh, Tbh = pb.get()
        for u in range(8):
            h = u % 4
            vh = cols[:, 640 + h * 64:640 + (h + 1) * 64]
            sl = slice((u % 4) * 128, (u % 4 + 1) * 128)
            us = slice(u * 64, (u + 1) * 64)
            k.op("pe", _mm(br[u // 4][0][0:64, sl], rho[:, us], ident, True, False), reads=[Tf, Tc], writes=[br[u // 4][1]])
            k.op("pe", _mm(br[u // 4][0][0:64, sl], Z[:, u, 0:64], XM[:, u, 128:256], False, True), reads=[TZh[u // 4], TXM[u]], writes=[br[u // 4][1]])
            k.op("pe", _mm(by[:, us], XM[:, u, 384:512], vh, True, False), reads=[TXM[u], Tco], writes=[Tby])
            k.op("pe", _mm(by[:, us], XM[:, u, 128:256], Z[:, u, 64:128], False, True), reads=[TXM[u], TZh[u // 4]], writes=[Tby])
            k.op("pe", _mm(bgm[0:64, us], Z[:, u, 0:64], bepn[:, us]), reads=[TZh[u // 4], Tf], writes=[Tbgm])
            k.op("pe", _mm(bh[0:64, us], kap[:, us], vh, True, False), reads=[Tf, Tco], writes=[Tbh])
            k.op("pe", _mm(bh[0:64, us], bepn[:, us], Z[:, u, 64:128], False, True), reads=[Tf, TZh[u // 4]], writes=[Tbh])
        for hb in range(2):
            k.op("act", _cp(Rbs[:, hb * 512:(hb + 1) * 512], br[hb][0][0:64, :]), reads=[br[hb][1]], writes=[TRbs])
        k.op("dve", _cp(Ybs[:], by[:, :]), reads=[Tby], writes=[TYbs])
        for u in range(8):
            us = slice(u * 64, (u + 1) * 64)
            k.op("dve", _stt(GHs[:, us], cst[0:64, C_I:C_I + 64], dcol[:, u:u + 1], bgm[0:64, us], ALU.mult, ALU.add), reads=[Tc, Tdc, Tbgm], writes=[TGHs])
        k.op("act", _cp(GHs[:, 512:1024], bh[0:64, :]), reads=[Tbh], writes=[TGHs])
        k.dma("sp", RB[i, :, :], Rbs[:], reads=[TRbs], writes=[TRB[i]])
        k.dma("sp", YB[i, :, :], Ybs[:], reads=[TYbs], writes=[TYB[i]])
        k.dma("sp", GH[i, :, :], GHs[:], reads=[TGHs], writes=[TGH[i]])

    for i in range(NT):
        phaseB(i)

    ST = k.sb("ST", [64, 512]); TST = Tok()
    k.op("dve", lambda e: e.memset(ST[:], 0.0), writes=[TST])
    GHc = [k.sb("GHc%d" % i, [64, 1024]) for i in range(2)]; TGc = [Tok(), Tok()]
    RBc = [k.sb("RBc%d" % i, [64, 1024]) for i in range(2)]; TRc = [Tok(), Tok()]
    yo = [k.sb("yo%d" % i, [128, 512]) for i in range(2)]; Tyo = [Tok(), Tok()]
    TYF = [Tok() for _ in range(NT)]; TYW = [Tok() for _ in range(NT)]
    order_f = list(range(NT))
    order_b = list(range(NCT - 1, -1, -1)) + list(range(NT - 1, NCT - 1, -1))
    for s in range(NT):
        tf, tb = order_f[s], order_b[s]
        g, Tg_ = GHc[s % 2], TGc[s % 2]
        rb, Trb_ = RBc[s % 2], TRc[s % 2]
        gv = g[:].rearrange("p (a c) -> p a c", a=2)
        k.dma("pool", gv[:, :, 0:256], GH[tf, :, :].rearrange("p (a c) -> p a c", a=2)[:, :, 0:256], reads=[TGH[tf]], writes=[Tg_])
        k.dma("pool", gv[:, :, 256:512], GH[tb, :, :].rearrange("p (a c) -> p a c", a=2)[:, :, 256:512], reads=[TGH[tb]], writes=[Tg_])
        k.dma("pool", rb[:, 0:512], RB[tf, :, 0:512], reads=[TRB[tf]], writes=[Trb_])
        k.dma("pool", rb[:, 512:1024], RB[tb, :, 512:1024], reads=[TRB[tb]], writes=[Trb_])
        by, Tby = pb.get()
        bs, Tbs = pb.get()
        for u in range(8):
            us = slice(u * 64, (u + 1) * 64)
            k.op("pe", _mm(by[:, us], rb[:, u * 128:(u + 1) * 128], ST[:, us]), reads=[Trb_, TST], writes=[Tby])
        for u in range(8):
            us = slice(u * 64, (u + 1) * 64)
            k.op("pe", _mm(bs[0:64, us], g[:, us], ST[:, us]), reads=[Tg_, TST], writes=[Tbs])
        k.op("act", _cp(yo[s % 2][:], by[:, :]), reads=[Tby], writes=[Tyo[s % 2]])
        k.op("dve", _tt(ST[:], bs[0:64, :], g[:, 512:1024], ALU.add), reads=[Tbs, Tg_, TST], writes=[TST])
        k.dma("sp", YF[tf, :, :], yo[s % 2][:, 0:256], reads=[Tyo[s % 2]], writes=[TYF[tf]])
        k.dma("sp", YW[tb, :, :], yo[s % 2][:, 256:512], reads=[Tyo[s % 2]], writes=[TYW[tb]])

    yf = k.sb("yf", [128, 256]); yw = k.sb("yw", [128, 256]); ybl = k.sb("ybl", [128, 512]); bvl = k.sb("bvl", [128, 512]); TDl = Tok()
    yy = k.sb("yy", [128, 256]); Tyy = Tok()
    m4 = k.sb("m4", [128, 4]); Tm4 = Tok()
    ywT = k.sb("ywT", [128, 2, 128]); TywT = Tok()
    for i in (range(NT) if need_ctx else range(NCT, NT)):
        k.dma("sp", yf[:], YF[i, :, :], reads=[TYF[i]], writes=[TDl])
        k.dma("sp", yw[:], YW[i, :, :], reads=[TYW[i]], writes=[TDl])
        k.dma("sp", ybl[:], YB[i, :, :], reads=[TYB[i]], writes=[TDl])
        k.dma("sp", bvl[:], BVG[i * 128:(i + 1) * 128, :], reads=[TBVG[i]], writes=[TDl])
        k.op("dve", _tt(yy[:], yf[:], yw[:], ALU.add), reads=[TDl], writes=[Tyy])
        k.op("dve", _tt(yy[:], yy[:], ybl[:, 0:256], ALU.add), reads=[TDl, Tyy], writes=[Tyy])
        k.op("dve", _tt(yy[:], yy[:], ybl[:, 256:512], ALU.add), reads=[TDl, Tyy], writes=[Tyy])
        k.op("dve", _rs(m4[:], h4(yy[:])), reads=[Tyy], writes=[Tm4])
        k.op("dve", _ts(m4[:], m4[:], 1.0 / 64, ALU.mult), reads=[Tm4], writes=[Tm4])
        k.op("dve", _tt(h4(yy[:]), h4(yy[:]), m4[:, :].unsqueeze(2).broadcast_to([128, 4, 64]), ALU.subtract), reads=[Tyy, Tm4], writes=[Tyy])
        k.op("dve", _tt(yf[:], yy[:], yy[:], ALU.mult), reads=[Tyy, TDl], writes=[TDl])
        k.op("dve", _rs(m4[:], h4(yf[:])), reads=[TDl, Tyy], writes=[Tm4])
        k.op("dve", _ts(m4[:], m4[:], 1.0 / 64, ALU.mult, 64e-5, ALU.add), reads=[Tm4], writes=[Tm4])
        rsqrt(k, m4[:], Tm4)
        k.op("dve", _tt(h4(yy[:]), h4(yy[:]), m4[:, :].unsqueeze(2).broadcast_to([128, 4, 64]), ALU.mult), reads=[Tyy, Tm4], writes=[Tyy])
        k.op("dve", _tt(yy[:], yy[:], lnw_bc, ALU.mult), reads=[Tyy, Tb2], writes=[Tyy])
        k.op("dve", _tt(yy[:], yy[:], lnb_bc, ALU.add), reads=[Tyy, Tb2], writes=[Tyy])
        k.op("dve", _tt(yy[:], yy[:], bvl[:, 0:256], ALU.add), reads=[Tyy, TDl], writes=[Tyy])
        k.op("dve", _tt(yw[:], yy[:], bvl[:, 256:512], ALU.mult), reads=[Tyy, TDl], writes=[TDl])
        bank, Tb = pb.get()
        for j in range(2):
            k.op("pe", _tr(bank[:, j * 128:(j + 1) * 128], yw[:, j * 128:(j + 1) * 128], ident), reads=[TDl, Tc], writes=[Tb])
        k.op("act", _cp(ywT[:], bank[:, 0:256].rearrange("p (a t) -> p a t", a=2)), reads=[Tb], writes=[TywT])
        k.dma("sp", mto(i)[256:512, :].rearrange("(a p) t -> p a t", p=128), ywT[:], reads=[TywT], writes=[TMTo])


def _bc(v, n=128):
    return np.ascontiguousarray(np.broadcast_to(np.asarray(v, np.float32)[None], (n,) + tuple(np.shape(v))))


def rope_tables(n_ctx, n_lat, grid_w=64):
    t = np.arange(n_lat)
    row = (t // grid_w).astype(np.float32)
    col = (t % grid_w).astype(np.float32)
    inv = (np.float32(10000.0) ** (-np.arange(16, dtype=np.float32) * np.float32(2.0) / np.float32(32))).astype(np.float32)
    ang = np.stack([row[:, None] * inv, col[:, None] * inv], 1).astype(np.float32)
    cos, sin = np.cos(ang).astype(np.float32), np.sin(ang).astype(np.float32)
    C = np.stack([cos, cos], 2).reshape(n_lat, 64)
    S = np.stack([-sin, sin], 2).reshape(n_lat, 64)
    out = np.zeros((n_ctx + n_lat, 2, 64), np.float32)
    out[:n_ctx, 0, :] = 1.0
    out[n_ctx:, 0, :] = C
    out[n_ctx:, 1, :] = S
    return out


def mixer_inputs(inp, l, b, g, rope, consts):
    f = lambda a: np.ascontiguousarray(np.asarray(a, np.float32))
    win = np.asarray(inp["w_in"][l]); R0 = 768
    hs = slice(256 * g, 256 * g + 256)
    sel = np.r_[256 * g:256 * g + 256, 512 + 64 * g:512 + 64 * g + 64, 640 + 64 * g:640 + 64 * g + 64,
                R0 + 1536:R0 + 1664,
                R0 + 256 * g:R0 + 256 * g + 256, R0 + 512 + 256 * g:R0 + 512 + 256 * g + 256,
                R0 + 1024 + 256 * g:R0 + 1024 + 256 * g + 256, R0 + 1664:R0 + 1792, R0 + 1792:R0 + 1920]
    tsel = sel[384:] - R0
    taps = np.asarray(inp["shift_taps"][l])[:, tsel]
    dup = np.stack([np.concatenate([np.asarray(inp["decay_up"][l][d])[:, hs], np.asarray(inp["decay_base"][l][d])[None, hs]], 0) for d in range(2)], 1)
    iup = np.stack([np.concatenate([np.asarray(inp["iclr_up"][l][d])[:, hs], np.asarray(inp["iclr_base"][l][d])[None, hs]], 0) for d in range(2)], 1)
    qg, kg = np.asarray(inp["q_gain"][l]), np.asarray(inp["k_gain"][l])
    b256 = np.stack([np.asarray(inp["k_k"][l])[hs], np.asarray(inp["k_a"][l])[hs], np.asarray(inp["r_k"][l]).reshape(-1)[hs],
                     np.asarray(inp["ln_x_w"][l])[hs], np.asarray(inp["ln_x_b"][l])[hs]], 0)
    return {
        "xc": f(np.concatenate([np.asarray(inp["ctx"][b]), np.asarray(inp["x"][b])], 0)),
        "cvT": f(np.stack([np.asarray(inp["c"][b]), np.asarray(inp["c_ctx"])], 1)),
        "modw": f(np.asarray(inp["mod_w"][l])[:, 0:2048]),
        "modb": f(np.asarray(inp["mod_b"][l])[0:2048].reshape(16, 128).T),
        "gain": f(np.asarray(inp["norm_mix"][l]).reshape(8, 128).T),
        "win": f(win[:, sel]),
        "bc64": _bc(np.stack([qg, qg, qg, qg, kg], 0)),
        "taps": _bc(taps),
        "bc256": _bc(b256),
        "dup": f(dup), "iup": f(iup),
        "gup": f(np.asarray(inp["gate_up"][l])[:, hs]),
        "rope": rope, "consts": consts,
    }


NEG = -1.0e30
GSZ = 3


def emit_ffn(k, pb, nc, A, NTL, nctx, NCH=128):
    NTOK = NTL * 128
    xr_t, Txr = A["xr_t"], A["Txr"]
    mta, mcol, TMTa, selr = A["mta"], A["mcol"], A["TMTa"], A["selr"]
    cvT, modw, modbc, modbr, gain = A["cvT"], A["modw_f"], A["modbc"], A["modbr"], A["gain_f"]
    woutd, wqd, kTd, UTd, Vd, constd = A["wout"], A["wq"], A["kT"], A["UT"], A["Vx"], A["consts"]
    xo_t, Txo = A["xo_t"], A["Txo"]

    es = ExitStack()
    with es:
        es_outer = k.es
        k.es = es
        cst = k.sb("cst", [128, 512]); Tc = Tok()
        k.dma("sp", cst[:], constd[:, 0:512], writes=[Tc])
        ident = cst[:, 0:128]
        identb = k.sb("identb", [128, 128], BF16); Tib = Tok()
        k.op("dve", _cp(identb[:], ident), reads=[Tc], writes=[Tib])
        es_main = k.es
        sel = k.sb("sel", [128, 2]); Tsel = Tok()
        k.dma("sp", sel[:], selr[:, :], writes=[Tsel])
        modc = k.sb("modc", [128, 16, 2]); Tmodc = Tok()
        gbc = k.sb("gbc", [128, 2, 2, 1024]); Tgbc = Tok()
        woutb = k.sb("woutb", [128, 8, D], BF16); wqb = k.sb("wqb", [128, 8, 2048], BF16); TW = Tok()
        kTb = k.sb("kTb", [128, 2, 128], BF16)
        es2 = ExitStack(); k.es = es2
        cv = k.sb("cv", [128, 8, 2]); Tcv = Tok()
        k.dma("sp", cv[:], cvT.rearrange("(kc p) two -> p kc two", p=128), writes=[Tcv])
        scv = k.sb("scv", [128, 8, 2]); Tscv = Tok()
        k.op("act", _act(scv[:], cv[:], AF.Silu), reads=[Tcv], writes=[Tscv])
        mwt = k.sb("mwt", [128, 8, 512]); Tmw = Tok()
        mbc = k.sb("mbc", [128, 16]); Tmb = Tok()
        k.dma("sp", mbc[:], modbc[:, :], writes=[Tmb])
        mbr = k.sb("mbr", [1, 2048]); Tmr = Tok()
        k.dma("sp", mbr[:], modbr[:, :], writes=[Tmr])
        gt = k.sb("gt", [128, 8]); Tg = Tok()
        k.dma("sp", gt[:], gain[:, :], writes=[Tg])
        grow = k.sb("grow", [1, 512]); Tgr = Tok()
        for cb in range(8):
            k.dma("sp", mwt[:], modw[:, cb * 512:(cb + 1) * 512].rearrange("(kc p) n -> p kc n", p=128), writes=[Tmw])
            if cb in (2, 3, 4, 5):
                bank, Tb = pb.get()
                for j in range(4):
                    for kc in range(8):
                        k.op("pe", _mm(bank[:, j * 2:j * 2 + 2], mwt[:, kc, j * 128:(j + 1) * 128], scv[:, kc, :], kc == 0, kc == 7),
                             reads=[Tmw, Tscv], writes=[Tb])
                c0 = (cb - 2) * 4
                k.op("dve", _tt(modc[:, c0:c0 + 4, :], bank[:, 0:8].rearrange("p (a b) -> p a b", a=4),
                                mbc[:, c0:c0 + 4].unsqueeze(2).broadcast_to([128, 4, 2]), ALU.add), reads=[Tb, Tmb], writes=[Tmodc])
            else:
                gi = 0 if cb < 2 else 1
                half = cb % 2
                for which in range(2):
                    bank, Tb = pb.get()
                    for kc in range(8):
                        k.op("pe", _mm(bank[0:1, :], scv[:, kc, which:which + 1], mwt[:, kc, :], kc == 0, kc == 7), reads=[Tmw, Tscv], writes=[Tb])
                    k.op("dve", _tt(grow[0:1, :], bank[0:1, :], mbr[0:1, gi * 1024 + half * 512:gi * 1024 + (half + 1) * 512], ALU.add),
                         reads=[Tb, Tmr], writes=[Tgr])
                    bank2, Tb2 = pb.get()
                    k.op("pe", _mm(bank2[:, :], cst[0:1, C_ONES:C_ONES + 128], grow[0:1, :]), reads=[Tc, Tgr], writes=[Tb2])
                    k.op("act", _cp(gbc[:, which, gi, half * 512:(half + 1) * 512], bank2[:, :]), reads=[Tb2], writes=[Tgbc])
        k.op("dve", _ts(modc[:, 8:16, :], modc[:, 8:16, :], 1.0, ALU.add), reads=[Tmodc], writes=[Tmodc])
        k.op("dve", _tt(modc[:, 8:16, :], modc[:, 8:16, :], gt[:, :].unsqueeze(2).broadcast_to([128, 8, 2]), ALU.mult), reads=[Tmodc, Tg], writes=[Tmodc])
        wst = k.sb("wst", [128, 2048]); Tws = Tok()
        for kc in range(8):
            k.dma("sp", wst[:, 0:1024], woutd[kc * 128:(kc + 1) * 128, :], writes=[Tws])
            k.op("act", _cp(woutb[:, kc, :], wst[:, 0:1024]), reads=[Tws], writes=[TW])
            k.dma("sp", wst[:], wqd[kc * 128:(kc + 1) * 128, :], writes=[Tws])
            k.op("dve", _cp(wqb[:, kc, :], wst[:]), reads=[Tws], writes=[TW])
        k.dma("sp", wst[:, 0:256], kTd.rearrange("p a n -> p (a n)"), writes=[Tws])
        k.op("dve", _cp(kTb[:].rearrange("p a n -> p (a n)"), wst[:, 0:256]), reads=[Tws], writes=[TW])
        k.barrier()
        es2.close()
        k.es = es_main

        xt = k.sb("xt", [128, D]); Tx = Tok()
        mt = k.sb("mt", [128, 8, 128]); Tmt = Tok()
        mtb = k.sb("mtb", [128, 8, 128], BF16); Tmtb = Tok()
        x1 = k.sb("x1", [128, D]); Tx1 = Tok()
        sq = k.sb("sq", [128, D]); Tsq = Tok()
        ss = k.sb("ss", [128, 1]); Tss = Tok()
        hfT = k.sb("hfT", [128, 8, GSZ * 128], BF16); ThT = [Tok() for _ in range(GSZ)]
        qT = k.sb("qT", [128, 16, 128], BF16); TqT = Tok()
        S = k.sb("S", [128, 16, 128]); TS = Tok()
        tmpS = k.sb("tmpS", [128, 128]); TtS = Tok()
        v16 = k.sb("v16", [128, 16, 16]); Tv = Tok()
        cand = k.sb("cand", [128, 8, 256]); Tcand = Tok()
        c16 = k.sb("c16", [128, 8, 16]); Tc16 = Tok()
        ec = k.sb("ec", [128, 8, 16]); Tec = Tok()
        zz = k.sb("zz", [128, 8]); Tzz = Tok()
        thr = k.sb("thr", [128, 8]); Tthr = Tok()
        S2 = [k.sb("S2_%d" % i, [128, 8, 128]) for i in range(GSZ)]
        E2t = k.sb("E2t", [128, 8, 128]); TE2t = Tok()
        E1 = [k.sb("E1_%d" % i, [128, 8, 128]) for i in range(GSZ)]
        TAU = [k.sb("TAU_%d" % i, [128, 8, 128]) for i in range(GSZ)]
        TG = [Tok() for _ in range(GSZ)]
        Uf = [k.sb("Uf%d" % i, [128, 8, 128]) for i in range(2)]; TUf = [Tok() for _ in range(2)]
        Ub = [k.sb("Ub%d" % i, [128, 8, 128], BF16) for i in range(3)]; TUb = [Tok() for _ in range(3)]
        Vf = [k.sb("Vf%d" % i, [128, D]) for i in range(2)]; TVf = [Tok() for _ in range(2)]
        Vb = [k.sb("Vb%d" % i, [128, D], BF16) for i in range(3)]; TVb = [Tok() for _ in range(3)]
        Af = [k.sb("Af%d" % i, [128, 8, 128], BF16) for i in range(3)]; TAf = [Tok() for _ in range(3)]
        Ab = [k.sb("Ab%d" % i, [128, 8, 128], BF16) for i in range(3)]; TAb = [Tok() for _ in range(3)]
        E2b = [k.sb("E2b_%d" % i, [128, 8, 128], BF16) for i in range(GSZ)]
        actTb = [k.sb("actTb%d" % i, [128, GSZ * 128], BF16) for i in range(3)]; TaT = [Tok() for _ in range(3)]
        Gsb = [k.sb("Gsb%d" % i, [128, GSZ * 128], BF16) for i in range(2)]; TGsb = [Tok(), Tok()]
        aTb = [k.sb("aTb%d" % i, [128, GSZ * 128], BF16) for i in range(2)]; TaTb = [Tok(), Tok()]
        mt2 = sq[:].rearrange("p (a t) -> p a t", a=8); Tmt2 = Tsq
        banks = pb.banks; Tbk = pb.toks

        def rr(n):
            rr.i += 1
            j = 6 + rr.i % 2
            return banks[j], Tbk[j]
        rr.i = 0

        def phase1(ti, slot):
            which = 1 if ti < nctx else 0
            k.dma("sp", xt[:], xr_t(ti), reads=(Txr if isinstance(Txr, list) else [Txr]), writes=[Tx])
            c0_, c1_ = mcol(0, ti), mcol(1, ti)
            k.dma("sp", mt[:], mta(c0_).rearrange("(kc p) t -> p kc t", p=128), reads=[TMTa], writes=[Tmt])
            k.dma("sp", mt2[:], mta(c1_).rearrange("(kc p) t -> p kc t", p=128), reads=[TMTa], writes=[Tmt2])
            k.op("dve", _ts(mt[:], mt[:], sel[:, 0:1], ALU.mult), reads=[Tmt, Tsel], writes=[Tmt])
            k.op("dve", _stt(mtb[:], mt2[:], sel[:, 1:2], mt[:], ALU.mult, ALU.add), reads=[Tmt, Tmt2, Tsel], writes=[Tmtb])
            for half in range(2):
                bank, Tb = rr(0)
                for kc in range(8):
                    k.op("pe", _mm(bank[:, :], mtb[:, kc, :], woutb[:, kc, half * 512:(half + 1) * 512], kc == 0, kc == 7), reads=[Tmtb, TW], writes=[Tb])
                hs = slice(half * 512, (half + 1) * 512)
                k.op("dve", _tt(x1[:, hs], bank[:, :], gbc[:, which, 0, hs], ALU.mult), reads=[Tb, Tgbc], writes=[Tx1])
            k.op("dve", _tt(x1[:], x1[:], xt[:], ALU.add), reads=[Tx1, Tx], writes=[Tx1])
            k.dma("sp", xo_t(ti), x1[:], reads=[Tx1], writes=[Txo[ti]])
            k.op("act", _act(sq[:], x1[:], AF.Square), reads=[Tx1], writes=[Tsq])
            k.op("dve", _rs(ss[:], sq[:]), reads=[Tsq], writes=[Tss])
            k.op("dve", _ts(ss[:], ss[:], 1.0 / D, ALU.mult, 1e-6, ALU.add), reads=[Tss], writes=[Tss])
            rsqrt(k, ss[:], Tss)
            k.op("act", _act(sq[:], x1[:], AF.Copy, scale=ss[:, 0:1]), reads=[Tx1, Tss, Tsq], writes=[Tsq])
            for half in range(2):
                bank, Tb = rr(0)
                for j in range(4):
                    c0 = (half * 4 + j) * 128
                    k.op("pe", _tr(bank[:, j * 128:(j + 1) * 128], sq[:, c0:c0 + 128], ident), reads=[Tsq, Tc], writes=[Tb])
                for j in range(4):
                    kc = half * 4 + j
                    k.op("act", _act(hfT[:, kc, slot * 128:(slot + 1) * 128], bank[:, j * 128:(j + 1) * 128], AF.Identity,
                                     scale=modc[:, 8 + kc, which:which + 1], bias=modc[:, kc, which:which + 1]),
                         reads=[Tb, Tmodc], writes=[ThT[slot]])
            for qb_ in range(4):
                bank, Tb = rr(0)
                for j in range(4):
                    blk = qb_ * 4 + j
                    for kc in range(8):
                        k.op("pe", _mm(bank[:, j * 128:(j + 1) * 128], wqb[:, kc, blk * 128:(blk + 1) * 128], hfT[:, kc, slot * 128:(slot + 1) * 128], kc == 0, kc == 7),
                             reads=[TW, ThT[slot]], writes=[Tb])
                k.op("dve", _cp(qT[:, qb_ * 4:(qb_ + 1) * 4, :], bank[:, :].rearrange("p (a t) -> p a t", a=4)), reads=[Tb], writes=[TqT])
            for qb_ in range(4):
                bank, Tb = rr(0)
                for j in range(4):
                    blk = qb_ * 4 + j
                    k.op("pe", _mm(bank[:, j * 128:(j + 1) * 128], qT[:, blk, :], kTb[:, blk % 2, :]), reads=[TqT, TW], writes=[Tb])
                k.op("act", _cp(S[:, qb_ * 4:(qb_ + 1) * 4, :], bank[:, :].rearrange("p (a t) -> p a t", a=4)), reads=[Tb], writes=[TS])
            for blk in range(16):
                k.op("dve", lambda e, blk=blk: e.max(out=v16[:, blk, 0:8], in_=S[:, blk, :]), reads=[TS], writes=[Tv])
                k.op("dve", lambda e, blk=blk: e.match_replace(out=tmpS[:], in_to_replace=v16[:, blk, 0:8], in_values=S[:, blk, :], imm_value=NEG),
                     reads=[TS, Tv], writes=[TtS])
                k.op("dve", lambda e, blk=blk: e.max(out=v16[:, blk, 8:16], in_=tmpS[:]), reads=[TtS], writes=[Tv])
            vv = v16[:].rearrange("p (h a) n -> p h a n", a=2)
            k.op("dve", _tt(cand[:].rearrange("p h (a b) -> p h a b", a=16), vv[:, :, 0, :].unsqueeze(3).broadcast_to([128, 8, 16, 16]),
                            vv[:, :, 1, :].unsqueeze(2).broadcast_to([128, 8, 16, 16]), ALU.add), reads=[Tv], writes=[Tcand])
            for h in range(8):
                k.op("dve", lambda e, h=h: e.max(out=c16[:, h, 0:8], in_=cand[:, h, :]), reads=[Tcand], writes=[Tc16])
                k.op("dve", lambda e, h=h: e.match_replace(out=cand[:, h, :], in_to_replace=c16[:, h, 0:8], in_values=cand[:, h, :], imm_value=NEG),
                     reads=[Tcand, Tc16], writes=[Tcand])
                k.op("dve", lambda e, h=h: e.max(out=c16[:, h, 8:16], in_=cand[:, h, :]), reads=[Tcand], writes=[Tc16])
            k.op("dve", _tt(ec[:], c16[:], c16[:, :, 0:1].broadcast_to([128, 8, 16]), ALU.subtract), reads=[Tc16], writes=[Tec])
            k.op("act", _act(ec[:], ec[:], AF.Exp), reads=[Tec], writes=[Tec])
            k.op("dve", _rs(zz[:], ec[:]), reads=[Tec], writes=[Tzz])
            k.op("dve", lambda e: e.reciprocal(zz[:], zz[:]), reads=[Tzz], writes=[Tzz])
            k.op("dve", _ts(thr[:], c16[:, :, 15], -1e-5, ALU.add), reads=[Tc16], writes=[Tthr])
            Sv = S[:].rearrange("p (h a) n -> p h a n", a=2)
            T_ = TG[slot]
            k.op("dve", _cp(S2[slot][:], Sv[:, :, 1, :]), reads=[TS], writes=[T_])
            k.op("dve", _tt(E2t[:], Sv[:, :, 1, :], vv[:, :, 1, 0:1].broadcast_to([128, 8, 128]), ALU.subtract), reads=[TS, Tv], writes=[TE2t])
            k.op("act", _act(E2b[slot][:], E2t[:], AF.Exp), reads=[TE2t], writes=[T_])
            k.op("dve", _tt(E1[slot][:], Sv[:, :, 0, :], vv[:, :, 0, 0:1].broadcast_to([128, 8, 128]), ALU.subtract), reads=[TS, Tv], writes=[T_])
            k.op("act", _act(E1[slot][:], E1[slot][:], AF.Exp), reads=[T_], writes=[T_])
            k.op("dve", _tt(E1[slot][:], E1[slot][:], zz[:, :].unsqueeze(2).broadcast_to([128, 8, 128]), ALU.mult), reads=[T_, Tzz], writes=[T_])
            k.op("dve", _tt(TAU[slot][:], thr[:, :].unsqueeze(2).broadcast_to([128, 8, 128]), Sv[:, :, 0, :], ALU.subtract), reads=[TS, Tthr], writes=[T_])

        ngroups = (NTL + GSZ - 1) // GSZ
        cnt = 0
        for gi in range(ngroups):
            tiles = list(range(gi * GSZ, min(NTL, (gi + 1) * GSZ)))
            ng = len(tiles)
            for slot, ti in enumerate(tiles):
                phase1(ti, slot)
            def Lstage(c):
                b3 = c % 3
                f2 = c % 2
                k.dma("sp", Uf[f2][:], UTd[:, c * 128:(c + 1) * 128].rearrange("(kc p) e -> p kc e", p=128), writes=[TUf[f2]])
                k.dma("sp", Vf[f2][:], Vd[c * 128:(c + 1) * 128, :], writes=[TVf[f2]])
                k.op("act", _cp(Ub[b3][:], Uf[f2][:]), reads=[TUf[f2]], writes=[TUb[b3]])
                k.op("act", _cp(Vb[b3][:], Vf[f2][:]), reads=[TVf[f2]], writes=[TVb[b3]])
                for kc in range(8):
                    k.op("pe", _mm(banks[6][:, 0:ng * 128], Ub[b3][:, kc, :], hfT[:, kc, 0:ng * 128], kc == 0, kc == 7), reads=[TUb[b3]] + ThT[:ng], writes=[Tbk[6]])
                k.op("act", _act(actTb[b3][:, 0:ng * 128], banks[6][:, 0:ng * 128], AF.Gelu), reads=[Tbk[6]], writes=[TaT[b3]])

            def Gstage(c):
                nonlocal cnt
                for slot in range(ng):
                    a4 = cnt % 3; cnt += 1
                    k.op("dve", _tt(Af[a4][:], S2[slot][:], TAU[slot][:, :, c:c + 1].broadcast_to([128, 8, 128]), ALU.is_ge), reads=[TG[slot]], writes=[TAf[a4]])
                    k.op("dve", _tt(Af[a4][:], Af[a4][:], E2b[slot][:], ALU.mult), reads=[TG[slot], TAf[a4]], writes=[TAf[a4]])
                    if slot == 2:
                        for h in range(8):
                            k.op("act", _act(Ab[a4][:, h, :], Af[a4][:, h, :], AF.Copy, scale=E1[slot][:, h, c:c + 1]), reads=[TG[slot], TAf[a4]], writes=[TAb[a4]])
                    else:
                        k.op("pool", _tt(Ab[a4][:], Af[a4][:], E1[slot][:, :, c:c + 1].broadcast_to([128, 8, 128]), ALU.mult), reads=[TG[slot], TAf[a4]], writes=[TAb[a4]])
                    for h in range(8):
                        k.op("pe", _mm(banks[7][:, slot * 128:(slot + 1) * 128], Ab[a4][:, h, :], identb[:], h == 0, h == 7), reads=[TAb[a4], Tib], writes=[Tbk[7]])
                k.op("act", _cp(Gsb[c % 2][:, 0:ng * 128], banks[7][:, 0:ng * 128]), reads=[Tbk[7]], writes=[TGsb[c % 2]])

            def Fstage(c):
                b2 = c % 2; b3 = c % 3
                k.op("dve", _tt(aTb[b2][:, 0:ng * 128], Gsb[b2][:, 0:ng * 128], actTb[b3][:, 0:ng * 128], ALU.mult), reads=[TGsb[b2], TaT[b3]], writes=[TaTb[b2]])
                for slot in range(ng):
                    for half in range(2):
                        j = slot * 2 + half
                        k.op("pe", _mm(banks[j][:, :], aTb[b2][:, slot * 128:(slot + 1) * 128], Vb[b3][:, half * 512:(half + 1) * 512], c == 0, c == NCH - 1),
                             reads=[TaTb[b2], TVb[b3]], writes=[Tbk[j]])

            Lstage(0)
            for c in range(NCH):
                if c + 1 < NCH:
                    Lstage(c + 1)
                Gstage(c)
                if c >= 1:
                    Fstage(c - 1)
            Fstage(NCH - 1)
            for slot, ti in enumerate(tiles):
                which = 1 if ti < nctx else 0
                k.dma("sp", xt[:], xo_t(ti), reads=[Txo[ti]], writes=[Tx])
                for half in range(2):
                    hs = slice(half * 512, (half + 1) * 512)
                    k.op("dve", _tt(x1[:, hs], banks[slot * 2 + half][:, :], gbc[:, which, 1, hs], ALU.mult), reads=[Tbk[slot * 2 + half], Tgbc], writes=[Tx1])
                k.op("dve", _tt(x1[:], x1[:], xt[:], ALU.add), reads=[Tx1, Tx], writes=[Tx1])
                k.dma("sp", xo_t(ti), x1[:], reads=[Tx1], writes=[Txo[ti]])
        k.barrier()
        k.es = es_outer
        print("ffn ninst", k.ninst)


def ffn_inputs(inp, l, xr, mixT, cvT, UT, consts):
    f = lambda a: np.ascontiguousarray(np.asarray(a, np.float32))
    mb = np.asarray(inp["mod_b"][l])
    return {
        "xr": f(xr), "mixT": f(mixT), "cvT": f(cvT),
        "modw": f(np.asarray(inp["mod_w"][l])[:, 2048:6144]),
        "modbc": f(mb[3072:5120].reshape(16, 128).T),
        "modbr": f(np.concatenate([mb[2048:3072], mb[5120:6144]])[None, :]),
        "gain": f(np.asarray(inp["norm_ffn"][l]).reshape(8, 128).T),
        "wout": f(inp["w_out"][l]), "wq": f(inp["peer_query"][l]),
        "kT": f(np.stack([np.asarray(inp["peer_subkeys1"][l]).T, np.asarray(inp["peer_subkeys2"][l]).T], 1)),
        "UT": UT, "Vx": f(inp["expert_v"][l]), "consts": consts,
    }


MIX_W = [("modw_m", [D, 2048]), ("modb_m", [128, 16]), ("gain_m", [128, 8]), ("win", [D, 1536]), ("bc64", [128, 5, 64]),
         ("taps", [128, 3, 1152]), ("bc256", [128, 5, 256]), ("dup", [65, 2, 256]), ("iup", [65, 2, 256]), ("gup", [128, 256])]
FFN_W = [("modw_f", [D, 4096]), ("modbc", [128, 16]), ("modbr", [1, 2048]), ("gain_f", [128, 8]), ("wout", [D, D]),
         ("wq", [D, 2048]), ("kT", [128, 2, 128]), ("UT", [D, 16384]), ("Vx", [16384, D])]


def build_fused(NCT, NLT, depth, groups):
    NT = NCT + NLT
    TT = NT * 128
    HC, HL = NCT * 64, NLT * 64
    NTOK = HC + HL
    NTR = NTOK // 128
    CW = 3 if NT % 3 == 0 else (2 if NT % 2 == 0 else 1)
    NMC = NT // CW
    XW = 2
    NXC = (NTR + XW - 1) // XW
    nc = bass.Bass("TRN2", target_bir_lowering=False)

    def din(name, shape):
        return nc.dram_tensor(name, list(shape), F32, kind="ExternalInput").ap()

    def dscr(name, shape):
        return nc.dram_tensor(name, list(shape), F32).ap()

    G = {"xc0": din("xc0", [TT, D]), "xr0": din("xr0", [NTOK, D]), "selr": din("selr", [128, 2]),
         "cvT": din("cvT", [D, 2]), "rope": din("rope", [TT, 2, 64]), "consts": din("consts", [128, NCONST])}
    W = []
    for l in range(depth):
        W.append({n: din("%s_%d" % (n, l), shp) for n, shp in MIX_W + FFN_W})
    out = nc.dram_tensor("out", [HL, D], F32, kind="ExternalOutput").ap()
    S = {"P": dscr("Pscr", [TT + 4, 1152]), "BVG": dscr("BVG", [TT, 512]), "GH": dscr("GH", [NT, 64, 1024]),
         "RB": dscr("RB", [NT, 64, 1024]), "YB": dscr("YB", [NT, 128, 512]), "YF": dscr("YF", [NT, 128, 256]),
         "YW": dscr("YW", [NT, 128, 256])}
    MToC = [dscr("MTo%d" % c, [512, CW * 128]) for c in range(NMC)]
    MTaC = [dscr("MTa%d" % c, [1024, CW * 128]) for c in range(NMC)]
    xrows = [min(XW, NTR - c * XW) * 128 for c in range(NXC)]
    XOC = [dscr("XO%d" % c, [xrows[c], D]) for c in range(NXC)]
    XGC = [dscr("XG%d" % c, [2 * xrows[c], D]) for c in range(NXC)]
    TMTo, TMTa, TXG = Tok(), Tok(), Tok()

    def mto(i):
        return MToC[i // CW][:, (i % CW) * 128:(i % CW + 1) * 128]

    def mta(col):
        i = col // 128
        return MTaC[i // CW][:, (i % CW) * 128:(i % CW + 1) * 128]

    def xo_int(t):
        return XOC[t // XW][(t % XW) * 128:(t % XW + 1) * 128, :]

    def xg(rk, t):
        c = t // XW
        r0 = rk * xrows[c] + (t % XW) * 128
        return XGC[c][r0:r0 + 128, :]

    es = ExitStack()
    with es:
        k = K(nc, es)
        pb = PB(k)
        Txo_prev = None
        for l in range(depth):
            last = l == depth - 1
            need_ctx = not last
            A = dict(G); A.update(W[l]); A.update(S)
            A["TMTo"] = TMTo
            A["mto"], A["mta"] = mto, mta
            if l == 0:
                A["xin"], A["Txc"] = (lambda i: G["xc0"][i * 128:(i + 1) * 128, :]), Tok()
            else:
                def xin(i):
                    if i < NCT:
                        return xg(i // (NCT // 2), i % (NCT // 2))
                    j = i - NCT
                    return xg(j // (NLT // 2), HC // 128 + j % (NLT // 2))
                A["xin"], A["Txc"] = xin, TXG
            emit_mixer(k, pb, nc, A, NCT, NLT, need_ctx)
            for c in range(NMC):
                k.collective("AllGather", MToC[c], MTaC[c], groups, reads=[TMTo], writes=[TMTa])
            nctx = HC // 128 if need_ctx else 0
            NTL = nctx + HL // 128
            if l == 0:
                A["xr_t"], A["Txr"] = (lambda ti: G["xr0"][ti * 128:(ti + 1) * 128, :]), Tok()
            else:
                off = 0 if need_ctx else HC // 128
                A["xr_t"], A["Txr"] = (lambda ti, off=off: xo_int(ti + off)), Txo_prev
            A["TMTa"] = TMTa

            def mcol(h, ti, nctx=nctx):
                if ti < nctx:
                    return h * HC + ti * 128
                return NCT * 128 + h * HL + (ti - nctx) * 128
            A["mcol"] = mcol
            Txo = [Tok() for _ in range(NTL)]
            A["Txo"] = Txo
            if last:
                A["xo_t"] = lambda ti: out[ti * 128:(ti + 1) * 128, :]
            else:
                if l > 0:
                    raise NotImplementedError("depth>2 needs ping-pong XO")
                A["xo_t"] = xo_int
            emit_ffn(k, pb, nc, A, NTL, nctx)
            if not last:
                for c in range(NXC):
                    k.collective("AllGather", XOC[c], XGC[c], groups, reads=Txo, writes=[TXG])
                Txo_prev = Txo
            else:
                k.finish(Txo)
        print("fused ninst", k.ninst)
    return nc


def fused_inputs(inp, b, r, NCT, NLT, rope, consts, UTs):
    f = lambda a: np.ascontiguousarray(np.asarray(a, np.float32))
    HC, HL = NCT * 64, NLT * 64
    depth = inp["w_in"].shape[0]
    m = {"xc0": f(np.concatenate([inp["ctx"][b], inp["x"][b]], 0)),
         "xr0": f(np.concatenate([inp["ctx"][b][r * HC:(r + 1) * HC], inp["x"][b][r * HL:(r + 1) * HL]], 0)),
         "selr": _bc(np.eye(2, dtype=np.float32)[r]),
         "cvT": f(np.stack([inp["c"][b], inp["c_ctx"]], 1)), "rope": rope, "consts": consts}
    perm = np.r_[0:256, 512:768, 256:512, 768:1024]
    for l in range(depth):
        mi = mixer_inputs(inp, l, b, r, rope, consts)
        for n_, src in (("modw_m", "modw"), ("modb_m", "modb"), ("gain_m", "gain"), ("win", "win"), ("bc64", "bc64"), ("taps", "taps"),
                        ("bc256", "bc256"), ("dup", "dup"), ("iup", "iup"), ("gup", "gup")):
            m["%s_%d" % (n_, l)] = mi[src]
        mb = np.asarray(inp["mod_b"][l])
        m["modw_f_%d" % l] = f(np.asarray(inp["mod_w"][l])[:, 2048:6144])
        m["modbc_%d" % l] = f(mb[3072:5120].reshape(16, 128).T)
        m["modbr_%d" % l] = f(np.concatenate([mb[2048:3072], mb[5120:6144]])[None, :])
        m["gain_f_%d" % l] = f(np.asarray(inp["norm_ffn"][l]).reshape(8, 128).T)
        m["wout_%d" % l] = f(np.asarray(inp["w_out"][l])[perm, :])
        m["wq_%d" % l] = f(inp["peer_query"][l])
        m["kT_%d" % l] = f(np.stack([np.asarray(inp["peer_subkeys1"][l]).T, np.asarray(inp["peer_subkeys2"][l]).T], 1))
        m["UT_%d" % l] = UTs[l]
        m["Vx_%d" % l] = f(inp["expert_v"][l])
    return m


def kernel(**inp):
    inp = {kk_: np.asarray(v) for kk_, v in inp.items()}
    B, SEQ_, _ = inp["x"].shape
    CTXL = inp["ctx"].shape[1]
    depth = inp["w_in"].shape[0]
    NCT, NLT = CTXL // 128, SEQ_ // 128
    consts = make_consts()
    rope = rope_tables(CTXL, SEQ_)
    n = 2 * B
    groups = [[2 * i, 2 * i + 1] for i in range(B)]
    nc = build_fused(NCT, NLT, depth, groups)
    UTs = [np.ascontiguousarray(inp["expert_u"][l].T) for l in range(depth)]
    maps = [fused_inputs(inp, c // 2, c % 2, NCT, NLT, rope, consts, UTs) for c in range(n)]
    res = run_bass_kernel_spmd(nc, maps, core_ids=list(range(n))).results
    HL = SEQ_ // 2
    out = np.empty((B, SEQ_, D), np.float32)
    for c in range(n):
        out[c // 2, (c % 2) * HL:(c % 2 + 1) * HL] = res[c]["out"]
    return out
```
